# Optimizing a Trainium2 kernel written in Bass

```python
import jax, jax.numpy as jnp
from jax import lax
import numpy as np

D_MODEL = 2048
BATCH = 4
SEQ = 4096
DEPTH = 2

N_A_LAYERS = DEPTH // 2
N_B_LAYERS = DEPTH - N_A_LAYERS
N_DENSE = (DEPTH + 1) // 2
N_MOE = DEPTH // 2

POOL_WINDOWS = (2, 4, 8, 16)
N_POOL_GROUPS = 4
POOL_GROUP_DIM = D_MODEL // N_POOL_GROUPS

HEAD_DIM = 128
N_HEADS = D_MODEL // HEAD_DIM
MOBA_BLOCK = 256
MOBA_TOPK = 3
MOBA_QCHUNK = 16
ROPE_THETA = 10000.0

D_FF = ((8 * D_MODEL // 3 + 255) // 256) * 256
N_EXPERTS = 8
MOE_TOPK = 2
D_FF_EXPERT = 7 * D_MODEL // 2
MOE_BLOCK = 512

DEEPNORM_ALPHA = (2.0 * DEPTH) ** 0.25
DEEPNORM_BETA = (8.0 * DEPTH) ** -0.25
LN_EPS = 1e-5
NEG = -1e30

kernel_name = 'yoco_pool_moba_moe_deepnorm'


def layer_norm(x, g, b):
    xf = x.astype(jnp.float32)
    mu = jnp.mean(xf, axis=-1, keepdims=True)
    var = jnp.mean(jnp.square(xf - mu), axis=-1, keepdims=True)
    y = (xf - mu) * lax.rsqrt(var + LN_EPS)
    return (y * g.astype(jnp.float32) + b.astype(jnp.float32)).astype(x.dtype)


def pool_mixer(x, w_groups, scale):
    B, S, D = x.shape
    xg = x.reshape(B, S, N_POOL_GROUPS, POOL_GROUP_DIM)
    csum = jnp.cumsum(xg.astype(jnp.float32), axis=1)
    t = jnp.arange(S)
    pooled = []
    for g, w in enumerate(POOL_WINDOWS):
        c = csum[:, :, g]
        lag = jnp.pad(c[:, :S - w], ((0, 0), (w, 0), (0, 0)))
        cnt = jnp.minimum(t + 1, w).astype(jnp.float32)[None, :, None]
        pooled.append((c - lag) / cnt)
    pooled = jnp.stack(pooled, axis=2)
    diff = (pooled - xg.astype(jnp.float32)).astype(x.dtype)
    y = jnp.einsum('bsgc,gcd->bsgd', diff, w_groups)
    return y.reshape(B, S, D) * scale


def rope_tables(S):
    inv = 1.0 / (ROPE_THETA ** (jnp.arange(0, HEAD_DIM, 2, dtype=jnp.float32) / HEAD_DIM))
    ang = jnp.arange(S, dtype=jnp.float32)[:, None] * inv[None, :]
    ang = jnp.concatenate([ang, ang], axis=-1)
    return jnp.cos(ang), jnp.sin(ang)


def apply_rope(t, cos, sin):
    tf = t.astype(jnp.float32)
    t1, t2 = jnp.split(tf, 2, axis=-1)
    rot = jnp.concatenate([-t2, t1], axis=-1)
    return (tf * cos + rot * sin).astype(t.dtype)


def split_heads(t):
    B, S, _ = t.shape
    return t.reshape(B, S, N_HEADS, HEAD_DIM).transpose(0, 2, 1, 3)


def shared_kv(h, w_kv, cos, sin):
    B, S, D = h.shape
    k, v = jnp.split(h @ w_kv, 2, axis=-1)
    k = apply_rope(split_heads(k), cos, sin)
    v = split_heads(v)
    nb = -(-S // MOBA_BLOCK)
    pad = nb * MOBA_BLOCK - S
    k = jnp.pad(k, ((0, 0), (0, 0), (0, pad), (0, 0)))
    v = jnp.pad(v, ((0, 0), (0, 0), (0, pad), (0, 0)))
    k_blocks = k.reshape(B, N_HEADS, nb, MOBA_BLOCK, HEAD_DIM)
    v_blocks = v.reshape(B, N_HEADS, nb, MOBA_BLOCK, HEAD_DIM)
    k_mean = jnp.mean(k_blocks.astype(jnp.float32), axis=3).astype(k.dtype)
    return k_blocks, v_blocks, k_mean


def moba_attention(x, w_q, w_o, k_blocks, v_blocks, k_mean, cos, sin):
    B, S, D = x.shape
    nb = k_blocks.shape[2]
    topk = min(MOBA_TOPK, nb)
    q = apply_rope(split_heads(x @ w_q), cos, sin)
    gate = jnp.einsum('bhsd,bhnd->bhsn', q, k_mean).astype(jnp.float32)
    q_blk = jnp.arange(S) // MOBA_BLOCK
    past = jnp.arange(nb)[None, :] < q_blk[:, None]
    gate = jnp.where(past, gate, NEG)
    _, sel = lax.top_k(gate, topk)
    scale = HEAD_DIM ** -0.5
    gather = jax.vmap(jax.vmap(lambda blocks, ix: blocks[ix]))
    sel_len = topk * MOBA_BLOCK

    def chunk(i):
        c0 = i * MOBA_QCHUNK
        qc = lax.dynamic_slice_in_dim(q, c0, MOBA_QCHUNK, axis=2)
        ic = lax.dynamic_slice_in_dim(sel, c0, MOBA_QCHUNK, axis=2)
        own = c0 // MOBA_BLOCK
        k_sel = gather(k_blocks, ic)
        v_sel = gather(v_blocks, ic)
        k_own = lax.dynamic_index_in_dim(k_blocks, own, axis=2, keepdims=False)
        v_own = lax.dynamic_index_in_dim(v_blocks, own, axis=2, keepdims=False)
        s_sel = jnp.einsum('bhqd,bhqjkd->bhqjk', qc, k_sel).astype(jnp.float32) * scale
        s_own = jnp.einsum('bhqd,bhkd->bhqk', qc, k_own).astype(jnp.float32) * scale
        qpos = c0 + jnp.arange(MOBA_QCHUNK)
        kpos = own * MOBA_BLOCK + jnp.arange(MOBA_BLOCK)
        s_own = jnp.where(kpos[None, :] <= qpos[:, None], s_own, NEG)
        slot_ok = jnp.arange(topk) < own
        s_sel = jnp.where(slot_ok[:, None], s_sel, NEG)
        s = jnp.concatenate([s_sel.reshape(B, N_HEADS, MOBA_QCHUNK, sel_len), s_own], axis=-1)
        p = jax.nn.softmax(s, axis=-1).astype(x.dtype)
        p_sel = p[..., :sel_len].reshape(B, N_HEADS, MOBA_QCHUNK, topk, MOBA_BLOCK)
        p_own = p[..., sel_len:]
        return (jnp.einsum('bhqjk,bhqjkd->bhqd', p_sel, v_sel)
                + jnp.einsum('bhqk,bhkd->bhqd', p_own, v_own))

    o = lax.map(chunk, jnp.arange(S // MOBA_QCHUNK))
    o = o.transpose(1, 0, 3, 2, 4).reshape(B, S, D)
    return o @ w_o


def swiglu(x, w_gate, w_up, w_down):
    return (jax.nn.silu(x @ w_gate) * (x @ w_up)) @ w_down


def moe_ffn(x, w_router, w_gate, w_up, w_down):
    B, S, D = x.shape
    T = B * S
    xf = x.reshape(T, D)
    logits = (xf @ w_router).astype(jnp.float32)
    top_logit, top_e = lax.top_k(logits, MOE_TOPK)
    gates = jax.nn.softmax(top_logit, axis=-1)
    a_exp = top_e.reshape(-1)
    a_tok = jnp.repeat(jnp.arange(T, dtype=jnp.int32), MOE_TOPK)
    a_gate = gates.reshape(-1)
    order = jnp.argsort(a_exp)
    s_exp, s_tok, s_gate = a_exp[order], a_tok[order], a_gate[order]
    counts = jnp.zeros((N_EXPERTS,), jnp.int32).at[a_exp].add(1)
    starts = jnp.cumsum(counts) - counts
    p_counts = (counts + MOE_BLOCK - 1) // MOE_BLOCK * MOE_BLOCK
    p_ends = jnp.cumsum(p_counts)
    p_starts = p_ends - p_counts
    n_assign = MOE_TOPK * T
    dest = p_starts[s_exp] + jnp.arange(n_assign, dtype=jnp.int32) - starts[s_exp]
    n_blocks = -(-n_assign // MOE_BLOCK) + N_EXPERTS
    P = n_blocks * MOE_BLOCK
    buf_tok = jnp.zeros((P,), jnp.int32).at[dest].set(s_tok)
    buf_gate = jnp.zeros((P,), jnp.float32).at[dest].set(s_gate)
    blk_start = jnp.arange(n_blocks, dtype=jnp.int32) * MOE_BLOCK
    blk_exp = jnp.minimum(jnp.searchsorted(p_ends, blk_start, side='right'), N_EXPERTS - 1)

    def expert_block(args):
        e, toks = args
        xb = xf[toks]
        return (jax.nn.silu(xb @ w_gate[e]) * (xb @ w_up[e])) @ w_down[e]

    out = lax.map(expert_block, (blk_exp, buf_tok.reshape(n_blocks, MOE_BLOCK)))
    out = out.reshape(P, D) * buf_gate[:, None].astype(x.dtype)
    y = jnp.zeros((T, D), x.dtype).at[buf_tok].add(out)
    return y.reshape(B, S, D)


def setup_inputs(seed: int = 0) -> dict:
    key = jax.random.key(seed)
    ks = jax.random.split(key, 20)
    f32 = jnp.float32
    n = lambda k, shp, s: jax.random.normal(k, shp, f32) * s
    D = D_MODEL
    w_k = n(ks[3], (D, D), D ** -0.5)
    w_v = n(ks[4], (D, D), D ** -0.5 * DEEPNORM_BETA)
    return {
        'x': jax.random.normal(ks[0], (BATCH, SEQ, D), f32),
        'pool_w': n(ks[1], (N_A_LAYERS, N_POOL_GROUPS, POOL_GROUP_DIM, POOL_GROUP_DIM), POOL_GROUP_DIM ** -0.5 * DEEPNORM_BETA),
        'pool_scale': 1.0 + n(ks[2], (N_A_LAYERS, D), 0.02),
        'w_kv': jnp.concatenate([w_k, w_v], axis=1),
        'moba_wq': n(ks[5], (N_B_LAYERS, D, D), D ** -0.5),
        'moba_wo': n(ks[6], (N_B_LAYERS, D, D), D ** -0.5 * DEEPNORM_BETA),
        'ffn_w_gate': n(ks[7], (N_DENSE, D, D_FF), D ** -0.5),
        'ffn_w_up': n(ks[8], (N_DENSE, D, D_FF), D ** -0.5),
        'ffn_w_down': n(ks[9], (N_DENSE, D_FF, D), D_FF ** -0.5 * DEEPNORM_BETA),
        'moe_router': n(ks[10], (N_MOE, D, N_EXPERTS), D ** -0.5),
        'moe_w_gate': n(ks[11], (N_MOE, N_EXPERTS, D, D_FF_EXPERT), D ** -0.5),
        'moe_w_up': n(ks[12], (N_MOE, N_EXPERTS, D, D_FF_EXPERT), D ** -0.5),
        'moe_w_down': n(ks[13], (N_MOE, N_EXPERTS, D_FF_EXPERT, D), D_FF_EXPERT ** -0.5 * DEEPNORM_BETA),
        'ln_mix_g': 1.0 + n(ks[14], (DEPTH, D), 0.02),
        'ln_mix_b': n(ks[15], (DEPTH, D), 0.02),
        'ln_ffn_g': 1.0 + n(ks[16], (DEPTH, D), 0.02),
        'ln_ffn_b': n(ks[17], (DEPTH, D), 0.02),
    }


def reference(x, pool_w, pool_scale, w_kv, moba_wq, moba_wo, ffn_w_gate, ffn_w_up, ffn_w_down,
              moe_router, moe_w_gate, moe_w_up, moe_w_down, ln_mix_g, ln_mix_b, ln_ffn_g, ln_ffn_b):
    S = x.shape[1]
    cos, sin = rope_tables(S)
    kv = None
    for l in range(DEPTH):
        if l < N_A_LAYERS:
            mix = pool_mixer(x, pool_w[l], pool_scale[l])
        else:
            b = l - N_A_LAYERS
            k_blocks, v_blocks, k_mean = kv
            mix = moba_attention(x, moba_wq[b], moba_wo[b], k_blocks, v_blocks, k_mean, cos, sin)
        x = layer_norm(DEEPNORM_ALPHA * x + mix, ln_mix_g[l], ln_mix_b[l])
        if l % 2 == 0:
            j = l // 2
            f = swiglu(x, ffn_w_gate[j], ffn_w_up[j], ffn_w_down[j])
        else:
            j = l // 2
            f = moe_ffn(x, moe_router[j], moe_w_gate[j], moe_w_up[j], moe_w_down[j])
        x = layer_norm(DEEPNORM_ALPHA * x + f, ln_ffn_g[l], ln_ffn_b[l])
        if l == N_A_LAYERS - 1:
            kv = shared_kv(x, w_kv, cos, sin)
    return x
```

```python
import numpy as np
from contextlib import ExitStack
import concourse.bass as bass
import concourse.mybir as mybir
from concourse.bass_utils import run_bass_kernel_spmd

F32 = mybir.dt.float32
BF16 = mybir.dt.bfloat16
I32 = mybir.dt.int32
ALU = mybir.AluOpType
AF = mybir.ActivationFunctionType
AX = mybir.AxisListType

D = 2048
SEQ = 4096
NB_ = 4
DFF = 5632
NE = 8
DFE = 7168
HD = 128
NH = 16
BLK = 256
ALPHA = float((2.0 * 2) ** 0.25)
EPS = 1e-5
WINS = (2, 4, 8, 16)
NCORE = 8
TPC = 2048
CAP = 640
NEGM = -30000.0
import os
ATT_PIPE = 2


def _box(ap):
    t = ap.tensor
    shp = list(t.shape)
    row = 1
    for s in shp[1:]:
        row *= int(s)
    off = int(ap.offset)
    dims = [(int(a), int(b)) for a, b in ap.ap]
    if "DRam" in type(t).__name__:
        lo = off
        hi = off + sum((c - 1) * abs(s) for s, c in dims) + 1
        return (t.name, 0, 1, lo, hi)
    pstep, pcnt = dims[0]
    p0 = off // row
    f0 = off % row
    if pstep == 0:
        pcnt = 1
    p1 = p0 + pcnt
    f1 = f0 + sum((c - 1) * abs(s) for s, c in dims[1:]) + 1
    return (t.name, p0, p1, f0, f1)


def _ovl(a, b):
    return a[1] < b[2] and b[1] < a[2] and a[3] < b[4] and b[3] < a[4]


def _covers(a, b):
    return a[1] <= b[1] and a[2] >= b[2] and a[3] <= b[3] and a[4] >= b[4]


class Sched:
    def __init__(self, nc, es, n_dma_slots=24):
        self.nc = nc
        self.E = {"pe": nc.tensor, "dve": nc.vector, "act": nc.scalar, "pool": nc.gpsimd, "sp": nc.sync}
        self.sem = {}
        self.cnt = {}
        for e in self.E:
            self.sem[e] = es.enter_context(nc.semaphore("s_" + e))
            self.cnt[e] = 0
        self.known = {e: {} for e in self.E}
        self.dq = {}
        for q in ("sp", "pool", "act"):
            sl = [es.enter_context(nc.semaphore("d_%s_%d" % (q, i))) for i in range(n_dma_slots)]
            self.dq[q] = {"sems": sl, "vals": [0] * n_dma_slots, "next": 0}
        self.semobj = {}
        for e in self.E:
            self.semobj[("c", e)] = self.sem[e]
        for q in self.dq:
            for i, s in enumerate(self.dq[q]["sems"]):
                self.semobj[("d", q, i)] = s
        self.track = {}
        self.n_wait = 0
        self.n_ins = 0

    def _deps(self, reads, writes):
        toks = []
        rb = [_box(a) for a in reads]
        wb = [_box(a) for a in writes]
        for b in rb:
            for ent in self.track.get(b[0], ()):
                if ent[1] == "w" and _ovl(ent[0], b):
                    toks.append(ent[2])
        for b in wb:
            for ent in self.track.get(b[0], ()):
                if _ovl(ent[0], b):
                    toks.append(ent[2])
        return toks, rb, wb

    def _record(self, rb, wb, tok):
        for b in wb:
            lst = self.track.setdefault(b[0], [])
            lst[:] = [e for e in lst if not _covers(b, e[0])]
            lst.append([b, "w", tok])
        for b in rb:
            lst = self.track.setdefault(b[0], [])
            lst[:] = [e for e in lst if not (e[1] == "r" and e[2][0] == tok[0] and _covers(b, e[0]))]
            lst.append([b, "r", tok])

    def _emit_waits(self, eng, toks):
        need = {}
        for k, v in toks:
            if v > need.get(k, 0):
                need[k] = v
        kn = self.known[eng]
        for k, v in need.items():
            if kn.get(k, 0) >= v:
                continue
            self.E[eng].wait_ge(self.semobj[k], v)
            self.n_wait += 1
            kn[k] = v

    def op(self, eng, fn, reads=(), writes=(), same_ok=False):
        toks, rb, wb = self._deps(reads, writes)
        if same_ok:
            toks = [t for t in toks if t[0] != ("c", eng)]
        self._emit_waits(eng, toks)
        ins = fn(self.E[eng])
        self.cnt[eng] += 1
        ins.then_inc(self.sem[eng], 1)
        self._record(rb, wb, (("c", eng), self.cnt[eng]))
        self.n_ins += 1
        return ins

    def dma(self, q, out, in_, fn=None, extra_reads=()):
        toks, rb, wb = self._deps([in_] + list(extra_reads), [out])
        d = self.dq[q]
        i = d["next"]
        d["next"] = (i + 1) % len(d["sems"])
        key = ("d", q, i)
        if d["vals"][i] > 0:
            toks.append((key, d["vals"][i]))
        self._emit_waits(q, toks)
        if fn is None:
            ins = self.E[q].dma_start(out=out, in_=in_)
        else:
            ins = fn(self.E[q])
        d["vals"][i] += 16
        ins.then_inc(d["sems"][i], 16)
        self._record(rb, wb, (key, d["vals"][i]))
        self.n_ins += 1

    def wait_all(self, eng):
        toks = []
        for e in self.E:
            if self.cnt[e] > 0:
                toks.append((("c", e), self.cnt[e]))
        for q, d in self.dq.items():
            for i, v in enumerate(d["vals"]):
                if v > 0:
                    toks.append((("d", q, i), v))
        self._emit_waits(eng, toks)


class Ctx:
    def __init__(self, nc, es, nring=5):
        self.nc = nc
        self.es = es
        self.S = Sched(nc, es)
        self.ring = [es.enter_context(nc.sbuf_tensor("ring%d" % i, [128, 4096], BF16)) for i in range(nring)]
        self.ring_i = 0
        self.psA = [es.enter_context(nc.psum_tensor("psA%d" % i, [128, 512], F32)) for i in range(4)]
        self.psT = [es.enter_context(nc.psum_tensor("psT%d" % i, [128, 1024], BF16)) for i in range(2)]
        self.psB = [es.enter_context(nc.psum_tensor("psB%d" % i, [128, 512], F32)) for i in range(2)]
        self.iA = 0
        self.iT = 0
        self.iB = 0
        self.identb = self.sb("identb", [128, 128], BF16)
        self.identf = self.sb("identf", [128, 128], F32)

    def sb(self, name, shape, dt):
        return self.es.enter_context(self.nc.sbuf_tensor("sb_" + name, shape, dt))

    def dram_in(self, name, shape, dt=F32):
        return self.nc.dram_tensor(name, list(shape), dt, kind="ExternalInput").ap()

    def dram_out(self, name, shape, dt=F32):
        return self.nc.dram_tensor(name, list(shape), dt, kind="ExternalOutput").ap()

    def nextA(self):
        p = self.psA[self.iA % 4]
        self.iA += 1
        return p

    def nextT(self):
        p = self.psT[self.iT % 2]
        self.iT += 1
        return p

    def nextB(self):
        p = self.psB[self.iB % 2]
        self.iB += 1
        return p

    def mm(self, out, lhsT, rhs, start, stop):
        self.S.op("pe", lambda e: e.matmul(out, lhsT, rhs, start=start, stop=stop),
                  reads=[lhsT, rhs], writes=[out], same_ok=True)

    def tr(self, out, in_, ident):
        self.S.op("pe", lambda e: e.transpose(out, in_, ident), reads=[in_, ident], writes=[out], same_ok=True)

    def tt(self, eng, out, a, b, op):
        self.S.op(eng, lambda e: e.tensor_tensor(out, a, b, op), reads=[a, b], writes=[out])

    def ts(self, eng, out, a, s1, s2, op0, op1=None):
        rd = [a] + [s for s in (s1, s2) if not isinstance(s, (int, float)) and s is not None]
        if op1 is None:
            self.S.op(eng, lambda e: e.tensor_scalar(out, a, s1, None, op0), reads=rd, writes=[out])
        else:
            self.S.op(eng, lambda e: e.tensor_scalar(out, a, s1, s2, op0, op1), reads=rd, writes=[out])

    def stt(self, eng, out, a, sc, b, op0, op1):
        rd = [a, b] + ([] if isinstance(sc, (int, float)) else [sc])
        self.S.op(eng, lambda e: e.scalar_tensor_tensor(out, a, sc, b, op0, op1), reads=rd, writes=[out])

    def cp(self, eng, out, a):
        if eng == "act":
            self.S.op("act", lambda e: e.activation(out, a, AF.Copy), reads=[a], writes=[out])
        else:
            self.S.op(eng, lambda e: e.tensor_copy(out, a), reads=[a], writes=[out])

    def act(self, out, a, func, bias=None, scale=None, accum_out=None):
        kw = {}
        rd = [a]
        wr = [out]
        if bias is not None:
            kw["bias"] = bias
            if not isinstance(bias, (int, float)):
                rd.append(bias)
        if scale is not None:
            kw["scale"] = scale
            if not isinstance(scale, (int, float)):
                rd.append(scale)
        if accum_out is not None:
            kw["accum_out"] = accum_out
            wr.append(accum_out)
        self.S.op("act", lambda e: e.activation(out, a, func, **kw), reads=rd, writes=wr)

    def slab(self, w2d, kc, ncols):
        r = self.ring[self.ring_i % len(self.ring)]
        self.ring_i += 1
        v = r[:, 0:kc * ncols].rearrange("p (c n) -> p c n", n=ncols)
        self.S.dma("pool", v, w2d.rearrange("(c p) n -> p c n", p=128))
        return v

    def bcast_load(self, dst, row_ap, q="sp"):
        self.S.dma(q, dst, row_ap.broadcast_to([128, int(row_ap.shape[-1])]))

    def layernorm(self, h, gbc, bbc, stats, mv, rstd):
        S = self.S
        for k in range(4):
            S.op("dve", lambda e: e.bn_stats(stats[:, k, :], h[:, k * 512:(k + 1) * 512]),
                 reads=[h[:, k * 512:(k + 1) * 512]], writes=[stats[:, k, :]])
        S.op("dve", lambda e: e.bn_aggr(mv[:, :], stats[:, :, :]), reads=[stats[:, :, :]], writes=[mv[:, :]])
        self.ts("dve", rstd[:, :], mv[:, 1:2], EPS, None, ALU.add)
        self.act(rstd[:, :], rstd[:, :], AF.Sqrt)
        self.S.op("dve", lambda e: e.reciprocal(rstd[:, :], rstd[:, :]), reads=[rstd[:, :]], writes=[rstd[:, :]])
        self.stt("dve", h, h, mv[:, 0:1], gbc, ALU.subtract, ALU.mult)
        self.stt("dve", h, h, rstd[:, 0:1], bbc, ALU.mult, ALU.add)

    def to_feature_major(self, xbf, xT, col0):
        for c0 in range(0, 16, 4):
            pt = self.nextT()
            for j in range(4):
                c = c0 + j
                self.tr(pt[:, j * 128:(j + 1) * 128], xbf[:, c * 128:(c + 1) * 128], self.identb[:, :])
            src = pt[:, 0:512].rearrange("p (c t) -> p c t", t=128)
            self.cp("act", xT[:, c0:c0 + 4, col0:col0 + 128], src)


def phase_A(C, NT, io, kms):
    S = C.S
    es = ExitStack()
    with es:
        _phase_A_body(C, NT, io, kms, es)
    C.barrier()


def _phase_A_body(C, NT, io, kms, es):
    S = C.S

    def sb(name, shape, dt):
        return es.enter_context(C.nc.sbuf_tensor("sbA_" + name, shape, dt))
    x_in = io["xc"]
    bands = sb("bands", [128, 3 * 4 * 2 * 128], F32)
    S.dma("sp", bands[:, :].rearrange("p (a t) -> p a t", t=128), io["bands"].rearrange("a p t -> p a t"))
    perm = sb("perm", [128, 128], F32)
    S.dma("sp", perm[:, :], io["perm"])
    poolw = sb("poolw", [128, 4 * 4 * 512], BF16)
    for g in range(4):
        S.dma("pool", poolw[:, g * 2048:(g + 1) * 2048].rearrange("p (c n) -> p c n", n=512),
              io["pool_w"][g].rearrange("(c p) n -> p c n", p=128))
    pw = poolw[:, :].rearrange("p (g c n) -> p g c n", g=4, c=4)
    bd = bands[:, :].rearrange("p (f g h t) -> p f g h t", f=3, g=4, h=2)
    gbc = sb("gbc", [128, D], F32)
    bbc = sb("bbc", [128, D], F32)
    sbc = sb("sbc", [128, D], F32)
    C.bcast_load(sbc[:, :], io["pool_scale"][0:1, :])
    xb = [sb("xb%d" % i, [128, D], F32) for i in range(2)]
    xm = [sb("xm%d" % i, [128, D], F32) for i in range(4)]
    xbf2 = [sb("xbf%d" % i, [128, D], BF16) for i in range(2)]
    xT = sb("xT", [128, 16 * 512], BF16)
    xTv = xT[:, :].rearrange("p (c t) -> p c t", t=512)
    hT = sb("hT", [128, 22 * 512], BF16)
    hTv = hT[:, :].rearrange("p (c t) -> p c t", t=512)
    dT = sb("dT", [128, 4 * 128], BF16)
    dTv = dT[:, :].rearrange("p (c t) -> p c t", t=128)
    stats = sb("stats", [128, 4, 6], F32)
    mv = sb("mv", [128, 2], F32)
    rstd = sb("rstd", [128, 1], F32)
    t32 = [sb("t32_%d" % i, [128, 512], F32) for i in range(2)]
    sg = t32
    kr = [sb("kr%d" % i, [128, 512], F32) for i in range(2)]
    krb = [sb("krb%d" % i, [128, 512], BF16) for i in range(2)]
    cosb = sb("cosb", [128, 512], F32)
    sinb = sb("sinb", [128, 512], F32)
    vb = [sb("vb%d" % i, [128, 512], BF16) for i in range(2)]
    kmv = kms[:, :].rearrange("p (h b) -> p h b", h=NH)

    for ti in range(NT):
        C.bcast_load(gbc[:, :], io["ln_mix_g"][0:1, :])
        C.bcast_load(bbc[:, :], io["ln_mix_b"][0:1, :])
        for s in range(4):
            q = ti * 4 + s
            seg = q // 16
            xc, xp = xb[q % 2], xb[(q - 1) % 2]
            r0 = 128 * (seg + 1) + q * 128
            if q % 16 == 0:
                S.dma("sp", xp[:, :], x_in[r0 - 128:r0, :])
            S.dma("sp", xc[:, :], x_in[r0:r0 + 128, :])
            f = (0 if seg == 0 else 2) if q % 16 == 0 else 1
            h = xm[s]
            for g in range(4):
                pd = C.nextA()
                for cc in range(4):
                    col = g * 512 + cc * 128
                    o = pd[:, cc * 128:(cc + 1) * 128]
                    C.mm(o, xp[:, col:col + 128], bd[:, f, g, 0, :], True, False)
                    C.mm(o, xc[:, col:col + 128], bd[:, f, g, 1, :], False, True)
                C.cp("act", dTv[:, :, :], pd[:, :].rearrange("p (c t) -> p c t", t=128))
                py = C.nextA()
                for cc in range(4):
                    C.mm(py[:, :], dTv[:, cc, :], pw[:, g, cc, :], cc == 0, cc == 3)
                hs = h[:, g * 512:(g + 1) * 512]
                C.tt("dve", hs, py[:, :], sbc[:, g * 512:(g + 1) * 512], ALU.mult)
                C.stt("dve", hs, xc[:, g * 512:(g + 1) * 512], ALPHA, hs, ALU.mult, ALU.add)
            C.layernorm(h[:, :], gbc[:, :], bbc[:, :], stats, mv, rstd)
            C.cp("act", xbf2[s % 2][:, :], h[:, :])
            if s > 0:
                C.to_feature_major(xbf2[(s - 1) % 2], xTv, (s - 1) * 128)
        C.to_feature_major(xbf2[1], xTv, 3 * 128)
        for half in range(2):
            for j in range(11):
                f0 = half * 2816 + j * 256
                wg = C.slab(io["ffn_w_gate"][:, f0:f0 + 256], 16, 256)
                wu = C.slab(io["ffn_w_up"][:, f0:f0 + 256], 16, 256)
                for m in range(2):
                    pg = C.nextA()
                    pu = C.nextA()
                    for c in range(16):
                        C.mm(pg[:, :], wg[:, c, m * 128:(m + 1) * 128], xTv[:, c, :], c == 0, c == 15)
                    for c in range(16):
                        C.mm(pu[:, :], wu[:, c, m * 128:(m + 1) * 128], xTv[:, c, :], c == 0, c == 15)
                    sgt = sg[(j * 2 + m) % 2]
                    C.act(sgt[:, :], pg[:, :], AF.Silu)
                    C.tt("dve", hTv[:, j * 2 + m, :], sgt[:, :], pu[:, :], ALU.mult)
            for n in range(4):
                pacc = [C.nextA() for _ in range(4)]
                for k in range(3):
                    kc = 8 if k < 2 else 6
                    r0 = half * 2816 + k * 1024
                    wd = C.slab(io["ffn_w_down"][r0:r0 + kc * 128, n * 512:(n + 1) * 512], kc, 512)
                    for s in range(4):
                        for c in range(kc):
                            fc = k * 8 + c
                            C.mm(pacc[s][:, :], hTv[:, fc, s * 128:(s + 1) * 128], wd[:, c, :], fc == 0, fc == 21)
                for s in range(4):
                    o = xm[s][:, n * 512:(n + 1) * 512]
                    if half == 0:
                        C.stt("dve", o, o, ALPHA, pacc[s][:, :], ALU.mult, ALU.add)
                    else:
                        C.tt("dve", o, o, pacc[s][:, :], ALU.add)
        C.bcast_load(gbc[:, :], io["ln_ffn_g"][0:1, :])
        C.bcast_load(bbc[:, :], io["ln_ffn_b"][0:1, :])
        for s in range(4):
            q = ti * 4 + s
            C.layernorm(xm[s][:, :], gbc[:, :], bbc[:, :], stats, mv, rstd)
            if q < 16:
                S.dma("sp", io["x1"][q * 128:(q + 1) * 128, :], xm[s][:, :])
            C.cp("act", xbf2[s % 2][:, :], xm[s][:, :])
            if s > 0:
                C.to_feature_major(xbf2[(s - 1) % 2], xTv, (s - 1) * 128)
        C.to_feature_major(xbf2[1], xTv, 3 * 128)
        S.dma("sp", cosb[:, :], io["cosT"][:, ti * 512:(ti + 1) * 512])
        S.dma("sp", sinb[:, :], io["sinT"][:, ti * 512:(ti + 1) * 512])
        wk_hold = [None]

        def kproj(hh):
            if hh % 2 == 0:
                wk_hold[0] = C.slab(io["w_kv"][:, hh * 128:hh * 128 + 256], 16, 256)
            wk = wk_hold[0]
            m = hh % 2
            pk = C.nextA()
            for c in range(16):
                C.mm(pk[:, :], wk[:, c, m * 128:(m + 1) * 128], xTv[:, c, :], c == 0, c == 15)
            return pk

        pend = kproj(0)
        for hh in range(NH):
            pk = pend
            if hh + 1 < NH:
                pend = kproj(hh + 1)
            t = t32[hh % 2]
            C.cp("act", t[:, :], pk[:, :])
            pr = C.nextA()
            C.mm(pr[:, :], perm[:, :], t[:, :], True, True)
            k_ = kr[hh % 2]
            C.tt("dve", k_[:, :], pr[:, :], sinb[:, :], ALU.mult)
            C.tt("dve", t[:, :], t[:, :], cosb[:, :], ALU.mult)
            C.tt("dve", k_[:, :], k_[:, :], t[:, :], ALU.add)
            kb = krb[hh % 2]
            C.cp("act", kb[:, :], k_[:, :])
            S.dma("sp", io["kT"][hh, :, ti * 512:(ti + 1) * 512], kb[:, :])
            S.op("dve", lambda e: e.tensor_reduce(kmv[:, hh, ti * 2:ti * 2 + 2],
                                                   k_[:, :].rearrange("p (b t) -> p b t", t=256), AX.X, ALU.add),
                 reads=[k_[:, :]], writes=[kmv[:, hh, ti * 2:ti * 2 + 2]])
        for n in range(4):
            pacc = [C.nextA() for _ in range(4)]
            for k in range(2):
                wv = C.slab(io["w_kv"][k * 1024:(k + 1) * 1024, 2048 + n * 512:2048 + (n + 1) * 512], 8, 512)
                for s in range(4):
                    for c in range(8):
                        cc = k * 8 + c
                        C.mm(pacc[s][:, :], xTv[:, cc, s * 128:(s + 1) * 128], wv[:, c, :], cc == 0, cc == 15)
            for s in range(4):
                v_ = vb[(n * 4 + s) % 2]
                C.cp("act", v_[:, :], pacc[s][:, :])
                q = ti * 4 + s
                S.dma("sp", io["v"][q * 128:(q + 1) * 128, n * 512:(n + 1) * 512], v_[:, :])
    C.ts("dve", kms[:, :], kms[:, :], 1.0 / 256.0, None, ALU.mult)


def make_bands(seg_start):
    out = np.zeros((3, 4, 2, 128, 128), np.float32)
    for f in range(3):
        start = (f == 0 and seg_start[0]) or (f == 2 and seg_start[1])
        for g, w in enumerate(WINS):
            for t in range(128):
                cnt = min(t + 1, w) if start else w
                for k in range(w):
                    tp = t - k
                    if tp >= 0:
                        out[f, g, 1, tp, t] += 1.0 / cnt
                    elif not start:
                        out[f, g, 0, 128 + tp, t] += 1.0 / cnt
                out[f, g, 1, t, t] -= 1.0
    return out.reshape(24, 128, 128)


def rope_tables_T(pos):
    inv = 1.0 / (10000.0 ** (np.arange(0, HD, 2, dtype=np.float32) / HD))
    ang = pos.astype(np.float32)[:, None] * inv[None, :].astype(np.float32)
    ang = np.concatenate([ang, ang], axis=-1)
    cos = np.cos(ang).astype(np.float32).T.copy()
    sin = np.sin(ang).astype(np.float32).T.copy()
    sin[:64, :] *= -1.0
    return cos, sin


def perm_matrix():
    P = np.zeros((128, 128), np.float32)
    for m in range(128):
        P[(m + 64) % 128, m] = 1.0
    return P


SCALE = float(HD ** -0.5)


def phase_B_attn(C, io, st):
    S = C.S
    es = ExitStack()
    with es:
        def sb(name, shape, dt):
            return es.enter_context(C.nc.sbuf_tensor("sbB_" + name, shape, dt))
        perm = sb("perm", [128, 128], F32)
        S.dma("sp", perm[:, :], io["perm"])
        tri = sb("tri", [128, 128], BF16)
        S.dma("sp", tri[:, :], io["tri"])
        kmb = sb("kmb", [128, NH * 16], BF16)
        C.cp("dve", kmb[:, :], st["kms"][:, :])
        kmbv = kmb[:, :].rearrange("p (h b) -> p h b", h=NH)
        vbias = sb("vbias", [128, 128], F32)
        v01 = sb("v01", [128, 128], F32)
        C.bcast_load(vbias[:, :], io["validb"])
        C.bcast_load(v01[:, :], io["valid01"])
        wr = sb("wr", [128, 16 * 8], F32)
        S.dma("sp", wr[:, :].rearrange("p (c e) -> p c e", e=8), io["moe_router"].rearrange("(c p) e -> p c e", p=128))
        wrv = wr[:, :].rearrange("p (c e) -> p c e", e=8)
        gbc = sb("gbc", [128, D], F32)
        bbc = sb("bbc", [128, D], F32)
        C.bcast_load(gbc[:, :], io["ln_mix_g"])
        C.bcast_load(bbc[:, :], io["ln_mix_b"])
        xm = [sb("xm%d" % i, [128, D], F32) for i in range(4)]
        xbf = sb("xbf", [128, D], BF16)
        ytmp = sb("ytmp", [128, D], F32)
        xT = sb("xT", [128, 16 * 512], BF16)
        xTv = xT[:, :].rearrange("p (c t) -> p c t", t=512)
        attn = [sb("attn%d" % i, [128, D], BF16) for i in range(4)]
        kbuf = [sb("kbuf%d" % i, [128, 15 * 256], BF16) for i in range(2)]
        vbuf = [sb("vbuf%d" % i, [128, 30 * 128], BF16) for i in range(2)]
        kob = [sb("kob%d" % i, [128, 512], BF16) for i in range(2)]
        vob = [sb("vob%d" % i, [128, 4 * 128], BF16) for i in range(2)]
        cosb = sb("cosb", [128, 512], F32)
        sinb = sb("sinb", [128, 512], F32)
        t32 = [sb("t32_%d" % i, [128, 512], F32) for i in range(2)]
        qf = [sb("qf%d" % i, [128, 512], F32) for i in range(2)]
        qb = [sb("qb%d" % i, [128, 512], BF16) for i in range(2)]
        gsb = [sb("gsb%d" % i, [128, 16], F32) for i in range(8)]
        m8 = [sb("m8_%d" % i, [128, 8], F32) for i in range(8)]
        selt = [sb("sel%d" % i, [128, 16], F32) for i in range(8)]
        biast = [sb("bias%d" % i, [128, 16], F32) for i in range(8)]
        rs = [sb("rs%d" % i, [128, 16], F32) for i in range(8)]
        tot = [sb("tot%d" % i, [128, 1], F32) for i in range(8)]
        pbuf = [sb("pbuf%d" % i, [128, 256], BF16) for i in range(4)]
        ptb = [sb("ptb%d" % i, [128, 256], BF16) for i in range(4)]
        stats = sb("stats", [128, 4, 6], F32)
        mv = sb("mv", [128, 2], F32)
        rstd = sb("rstd", [128, 1], F32)
        x2Tf = sb("x2Tf", [128, 16 * 128], F32)
        x2Tv = x2Tf[:, :].rearrange("p (c t) -> p c t", t=128)
        lg = sb("lg", [128, 8], F32)
        rm8 = sb("rm8", [128, 8], F32)
        rex = sb("rex", [128, 8], F32)
        rneg = sb("rneg", [128, 1], F32)
        rden = sb("rden", [128, 1], F32)
        kp = 0
        it = 0
        for gi in range(4):
            for s in range(4):
                q = gi * 4 + s
                S.dma("sp", xm[s][:, :], io["x1"][q * 128:(q + 1) * 128, :])
                C.cp("act", xbf[:, :], xm[s][:, :])
                C.to_feature_major(xbf, xTv, s * 128)
            S.dma("sp", cosb[:, :], io["cosT"][:, gi * 512:(gi + 1) * 512])
            S.dma("sp", sinb[:, :], io["sinT"][:, gi * 512:(gi + 1) * 512])
            n0 = 2 * gi + 1
            cands = list(range(0, n0)) + list(range(8, 16))
            ncand = len(cands)
            wq_hold = [None]

            def pre(h):
                m = h % 2
                if m == 0:
                    wq_hold[0] = C.slab(io["moba_wq"][:, h * 128:h * 128 + 256], 16, 256)
                wq = wq_hold[0]
                kh = kbuf[m]
                vh = vbuf[m][:, :].rearrange("p (c d) -> p c d", d=128)
                ko = kob[m]
                vo = vob[m][:, :].rearrange("p (c d) -> p c d", d=128)
                S.dma("sp", kh[:, 0:n0 * 256], io["kT"][h, :, 0:n0 * 256])
                S.dma("sp", kh[:, n0 * 256:ncand * 256], io["kT"][h, :, TPC:2 * TPC])
                S.dma("sp", vh[:, 0:n0 * 2, :],
                      io["v"][0:n0 * 256, h * 128:(h + 1) * 128].rearrange("(c p) d -> p c d", p=128))
                S.dma("sp", vh[:, n0 * 2:ncand * 2, :],
                      io["v"][TPC:2 * TPC, h * 128:(h + 1) * 128].rearrange("(c p) d -> p c d", p=128))
                S.dma("sp", ko[:, :], io["kT"][h, :, gi * 512:(gi + 1) * 512])
                S.dma("sp", vo[:, :, :],
                      io["v"][gi * 512:(gi + 1) * 512, h * 128:(h + 1) * 128].rearrange("(c p) d -> p c d", p=128))
                pq = C.nextA()
                for c in range(16):
                    C.mm(pq[:, :], wq[:, c, m * 128:(m + 1) * 128], xTv[:, c, :], c == 0, c == 15)
                t = t32[m]
                C.cp("act", t[:, :], pq[:, :])
                pr = C.nextA()
                C.mm(pr[:, :], perm[:, :], t[:, :], True, True)
                q_ = qf[m]
                C.tt("dve", q_[:, :], pr[:, :], sinb[:, :], ALU.mult)
                C.tt("dve", t[:, :], t[:, :], cosb[:, :], ALU.mult)
                qbt = qb[m]
                C.tt("dve", qbt[:, :], q_[:, :], t[:, :], ALU.add)
                pgt = C.nextA()
                for r in range(4):
                    C.mm(pgt[:, r * 16:(r + 1) * 16], qbt[:, r * 128:(r + 1) * 128], kmbv[:, h, :], True, True)
                for r in range(4):
                    lb = gi * 2 + r // 2
                    k8 = m * 4 + r
                    g_, m8_, sl_, bi_ = gsb[k8], m8[k8], selt[k8], biast[k8]
                    C.tt("dve", g_[:, :], pgt[:, r * 16:(r + 1) * 16], vbias[:, lb * 16:(lb + 1) * 16], ALU.add)
                    S.op("dve", lambda e: e.max(m8_[:, :], g_[:, :]), reads=[g_[:, :]], writes=[m8_[:, :]])
                    C.ts("dve", sl_[:, :], g_[:, :], m8_[:, 2:3], None, ALU.is_ge)
                    C.tt("dve", sl_[:, :], sl_[:, :], v01[:, lb * 16:(lb + 1) * 16], ALU.mult)
                    C.ts("dve", bi_[:, :], sl_[:, :], 1.0, -NEGM, ALU.subtract, ALU.mult)

            def stage_a(w):
                h, r, m = w["h"], w["r"], w["h"] % 2
                qt = qb[m][:, r * 128:(r + 1) * 128]
                k8 = m * 4 + r
                ps = C.nextA()
                p = pbuf[w["i"] % 4]
                w["p"] = p
                if w["kind"] == "past":
                    cs, c = w["cs"], w["c"]
                    C.mm(ps[:, 0:256], qt, kbuf[m][:, cs * 256:(cs + 1) * 256], True, True)
                    C.act(p[:, :], ps[:, 0:256], AF.Exp, bias=biast[k8][:, c:c + 1], scale=SCALE,
                          accum_out=rs[k8][:, cs:cs + 1])
                else:
                    rr = r % 2
                    nk = rr + 1
                    boff = (r // 2) * 256
                    ko = kob[m]
                    for kc in range(nk):
                        o = ps[:, kc * 128:(kc + 1) * 128]
                        if kc == rr:
                            C.mm(o, qt, ko[:, boff + kc * 128:boff + (kc + 1) * 128], True, False)
                            C.mm(o, C.identb[:, :], tri[:, :], False, True)
                        else:
                            C.mm(o, qt, ko[:, boff + kc * 128:boff + (kc + 1) * 128], True, True)
                    C.act(p[:, 0:nk * 128], ps[:, 0:nk * 128], AF.Exp, scale=SCALE, accum_out=rs[k8][:, ncand:ncand + 1])

            def stage_b(w):
                nk = 2 if w["kind"] == "past" else (w["r"] % 2 + 1)
                i = w["i"]
                pt = C.psT[i % 2][:, ((i // 2) % 4) * 256:((i // 2) % 4) * 256 + 256]
                p = w["p"]
                for kc in range(nk):
                    C.tr(pt[:, kc * 128:(kc + 1) * 128], p[:, kc * 128:(kc + 1) * 128], C.identb[:, :])
                pts = ptb[i % 4]
                C.cp("dve", pts[:, 0:nk * 128], pt[:, 0:nk * 128])
                w["pts"] = pts

            def stage_c(w):
                h, r, m = w["h"], w["r"], w["h"] % 2
                k8 = m * 4 + r
                po = C.psB[(h * 4 + r) % 2]
                pts = w["pts"]
                if w["kind"] == "past":
                    vh = vbuf[m][:, :].rearrange("p (c d) -> p c d", d=128)
                    for kc in range(2):
                        C.mm(po[:, 0:128], pts[:, kc * 128:(kc + 1) * 128], vh[:, w["cs"] * 2 + kc, :],
                             w["cs"] == 0 and kc == 0, False)
                else:
                    nk = r % 2 + 1
                    vo = vob[m][:, :].rearrange("p (c d) -> p c d", d=128)
                    for kc in range(nk):
                        C.mm(po[:, 0:128], pts[:, kc * 128:(kc + 1) * 128], vo[:, (r // 2) * 2 + kc, :], False, kc == nk - 1)
                    tot_ = tot[k8]
                    S.op("dve", lambda e: e.tensor_reduce(tot_[:, :], rs[k8][:, 0:ncand + 1], AX.X, ALU.add),
                         reads=[rs[k8][:, 0:ncand + 1]], writes=[tot_[:, :]])
                    S.op("dve", lambda e: e.reciprocal(tot_[:, :], tot_[:, :]), reads=[tot_[:, :]], writes=[tot_[:, :]])
                    C.ts("dve", attn[r][:, h * 128:(h + 1) * 128], po[:, 0:128], tot_[:, 0:1], None, ALU.mult)

            pre(0)
            pend_b = None
            pend_c = None
            for h in range(NH):
                items = []
                for r in range(4):
                    for cs, c in enumerate(cands):
                        items.append(dict(h=h, r=r, kind="past", cs=cs, c=c))
                    items.append(dict(h=h, r=r, kind="own"))
                for wi, w in enumerate(items):
                    if wi == 3 and h + 1 < NH:
                        pre(h + 1)
                    w["i"] = kp
                    kp += 1
                    if ATT_PIPE == 0:
                        stage_a(w)
                        stage_b(w)
                        stage_c(w)
                        continue
                    stage_a(w)
                    if ATT_PIPE == 1:
                        if pend_b is not None:
                            stage_b(pend_b)
                            stage_c(pend_b)
                        pend_b = w
                        continue
                    if pend_b is not None:
                        stage_b(pend_b)
                    if pend_c is not None:
                        stage_c(pend_c)
                    pend_c = pend_b
                    pend_b = w
            if pend_b is not None:
                stage_b(pend_b)
                if pend_c is not None:
                    stage_c(pend_c)
                stage_c(pend_b)
            pend_b = None
            pend_c = None
            for s in range(4):
                C.to_feature_major(attn[s], xTv, s * 128)
            for n in range(4):
                pacc = [C.nextA() for _ in range(4)]
                for k in range(2):
                    wo = C.slab(io["moba_wo"][k * 1024:(k + 1) * 1024, n * 512:(n + 1) * 512], 8, 512)
                    for s in range(4):
                        for c in range(8):
                            hh = k * 8 + c
                            C.mm(pacc[s][:, :], xTv[:, hh, s * 128:(s + 1) * 128], wo[:, c, :], hh == 0, hh == 15)
                for s in range(4):
                    o = xm[s][:, n * 512:(n + 1) * 512]
                    C.stt("dve", o, o, ALPHA, pacc[s][:, :], ALU.mult, ALU.add)
            for s in range(4):
                q = gi * 4 + s
                C.layernorm(xm[s][:, :], gbc[:, :], bbc[:, :], stats, mv, rstd)
                C.cp("act", xbf[:, :], xm[s][:, :])
                S.dma("sp", io["x2b"][q * 128:(q + 1) * 128, :], xbf[:, :])
                C.ts("dve", ytmp[:, :], xm[s][:, :], ALPHA, None, ALU.mult)
                S.dma("sp", io["ybuf"][q * 128:(q + 1) * 128, :], ytmp[:, :])
                for c0 in range(0, 16, 4):
                    pt = C.nextA()
                    for j in range(4):
                        c = c0 + j
                        C.tr(pt[:, j * 128:(j + 1) * 128], xm[s][:, c * 128:(c + 1) * 128], C.identf[:, :])
                    C.cp("act", x2Tv[:, c0:c0 + 4, :], pt[:, :].rearrange("p (c t) -> p c t", t=128))
                pl = C.nextA()
                for c in range(16):
                    C.mm(pl[:, 0:8], x2Tv[:, c, :], wrv[:, c, :], c == 0, c == 15)
                C.cp("dve", lg[:, :], pl[:, 0:8])
                S.op("dve", lambda e: e.max(rm8[:, :], lg[:, :]), reads=[lg[:, :]], writes=[rm8[:, :]])
                mf = st["maskf"][:, q * 8:(q + 1) * 8]
                C.ts("dve", mf, lg[:, :], rm8[:, 1:2], None, ALU.is_ge)
                C.cp("dve", st["maskb"][:, q * 8:(q + 1) * 8], mf)
                C.ts("dve", rneg[:, :], rm8[:, 0:1], -1.0, None, ALU.mult)
                C.act(rex[:, :], lg[:, :], AF.Exp, bias=rneg[:, 0:1], scale=1.0)
                C.tt("dve", rex[:, :], rex[:, :], mf, ALU.mult)
                S.op("dve", lambda e: e.tensor_reduce(rden[:, :], rex[:, :], AX.X, ALU.add), reads=[rex[:, :]], writes=[rden[:, :]])
                S.op("dve", lambda e: e.reciprocal(rden[:, :], rden[:, :]), reads=[rden[:, :]], writes=[rden[:, :]])
                C.ts("dve", st["gate"][:, q * 8:(q + 1) * 8], rex[:, :], rden[:, 0:1], None, ALU.mult)
    C.barrier()


def phase_B_moe(C, io, st):
    S = C.S
    es = ExitStack()
    NCH = CAP // 128
    with es:
        def sb(name, shape, dt):
            return es.enter_context(C.nc.sbuf_tensor("sbM_" + name, shape, dt))
        onesb = sb("onesb", [128, 128], BF16)
        ustr = sb("ustr", [128, 128], BF16)
        S.op("dve", lambda e: e.memset(onesb[:, :], 1.0), writes=[onesb[:, :]])
        S.dma("sp", ustr[:, :], io["ustrict"])
        iot = sb("iot", [128, CAP], F32)
        C.bcast_load(iot[:, :], io["iota"])
        tokf = sb("tokf", [128, 16], F32)
        S.dma("sp", tokf[:, :], io["tokid"])
        slotm = sb("slotm", [128, 16 * 8], F32)
        tmp8 = sb("tmp8", [128, 8], F32)
        tg = sb("tg", [128, 16 * 8 * 3], F32)
        tgv = tg[:, :].rearrange("p (i e k) -> p i e k", i=16, e=8)
        maskb = st["maskb"][:, :].rearrange("p (i e) -> p i e", e=8)
        maskf = st["maskf"][:, :].rearrange("p (i e) -> p i e", e=8)
        gatev = st["gate"][:, :].rearrange("p (i e) -> p i e", e=8)
        for i in range(16):
            pr = C.nextA()
            for i2 in range(i):
                C.mm(pr[:, 0:8], onesb[:, :], maskb[:, i2, :], i2 == 0, False)
            C.mm(pr[:, 0:8], ustr[:, :], maskb[:, i, :], i == 0, True)
            C.stt("dve", tmp8[:, :], pr[:, 0:8], 1.0, maskf[:, i, :], ALU.add, ALU.mult)
            C.ts("dve", slotm[:, i * 8:(i + 1) * 8], tmp8[:, :], -1.0, None, ALU.add)
        for e_ in range(8):
            C.cp("dve", tgv[:, :, e_, 0], tokf[:, :])
        C.cp("dve", tgv[:, :, :, 1], gatev)
        S.op("dve", lambda e: e.memset(tgv[:, :, :, 2], 1.0), writes=[tgv[:, :, :, 2]])
        selm = [sb("selm%d" % i, [128, CAP], F32) for i in range(3)]
        tab = sb("tab", [128, 8 * NCH * 4], F32)
        tabv = tab[:, :].rearrange("p (e k c) -> p e k c", e=8, k=NCH)
        idxf = sb("idxf", [128, 8 * NCH], F32)
        idxi = sb("idxi", [128, 8 * NCH], I32)
        gsl = sb("gsl", [128, 8 * NCH], F32)
        k3 = 0
        for e_ in range(8):
            ptab = (C.psA + C.psB)[0:NCH]
            for i in range(16):
                sm = selm[k3 % 3]
                k3 += 1
                C.ts("dve", sm[:, :], iot[:, :], slotm[:, i * 8 + e_:i * 8 + e_ + 1], None, ALU.is_equal)
                for k in range(NCH):
                    C.mm(ptab[k][:, 0:3], sm[:, k * 128:(k + 1) * 128], tgv[:, i, e_, :], i == 0, i == 15)
            for k in range(NCH):
                C.cp("dve", tabv[:, e_, k, 0:3], ptab[k][:, 0:3])
        tv = tab[:, :].rearrange("p (j c) -> p j c", c=4)
        C.ts("dve", idxf[:, :], tv[:, :, 2], -4096.0, 4096.0, ALU.mult, ALU.add)
        C.tt("dve", idxf[:, :], idxf[:, :], tv[:, :, 0], ALU.add)
        C.cp("dve", idxi[:, :], idxf[:, :])
        C.cp("dve", gsl[:, :], tv[:, :, 1])
        if "dbg_idx" in io:
            S.dma("sp", io["dbg_idx"], idxf[:, :])
            S.dma("sp", io["dbg_gsl"], gsl[:, :])
            S.dma("sp", io["dbg_slot"], slotm[:, :])
            S.dma("sp", io["dbg_tab"], tab[:, :])
        XeT = sb("XeT", [128, 16 * CAP], BF16)
        XeTv = XeT[:, :].rearrange("p (c t) -> p c t", t=CAP)
        hTe = sb("hTe", [128, 56 * CAP], BF16)
        hTev = hTe[:, :].rearrange("p (c t) -> p c t", t=CAP)
        xg = [sb("xg%d" % i, [128, D], BF16) for i in range(2)]
        oe = [sb("oe%d" % i, [128, D], F32) for i in range(NCH)]
        sg = [sb("sg%d" % i, [128, CAP], F32) for i in range(2)]
        for i in range(2):
            S.op("dve", lambda e: e.memset(xg[i][:, :], 0.0), writes=[xg[i][:, :]])
        banks = C.psA + C.psB
        bi = 0
        breg = C.nc.gpsimd.alloc_register("bcreg")
        C.nc.gpsimd.reg_mov(breg, TPC - 1)
        N0 = 512
        N1 = CAP - 512
        for e_ in range(8):
            for k in range(NCH):
                g = xg[k % 2]
                j = e_ * NCH + k
                S.dma("pool", g[:, :], io["x2b"], fn=lambda e: e.indirect_dma_start(
                    out=g[:, :], out_offset=None, in_=io["x2b"][:, :],
                    in_offset=bass.IndirectOffsetOnAxis(ap=idxi[:, j:j + 1], axis=0),
                    bounds_check=breg, oob_is_err=False), extra_reads=[idxi[:, j:j + 1]])
                C.to_feature_major(g, XeTv, k * 128)
            for jf in range(28):
                wg = C.slab(io["moe_w_gate"][e_, :, jf * 256:(jf + 1) * 256], 16, 256)
                wu = C.slab(io["moe_w_up"][e_, :, jf * 256:(jf + 1) * 256], 16, 256)
                for m in range(2):
                    pg0, pg1, pu0, pu1 = [banks[(bi + z) % 6] for z in range(4)]
                    bi += 4
                    for c in range(16):
                        C.mm(pg0[:, 0:N0], wg[:, c, m * 128:(m + 1) * 128], XeTv[:, c, 0:N0], c == 0, c == 15)
                        C.mm(pg1[:, 0:N1], wg[:, c, m * 128:(m + 1) * 128], XeTv[:, c, N0:CAP], c == 0, c == 15)
                    for c in range(16):
                        C.mm(pu0[:, 0:N0], wu[:, c, m * 128:(m + 1) * 128], XeTv[:, c, 0:N0], c == 0, c == 15)
                        C.mm(pu1[:, 0:N1], wu[:, c, m * 128:(m + 1) * 128], XeTv[:, c, N0:CAP], c == 0, c == 15)
                    fch = jf * 2 + m
                    sgt = sg[fch % 2]
                    C.act(sgt[:, 0:N0], pg0[:, 0:N0], AF.Silu)
                    C.act(sgt[:, N0:CAP], pg1[:, 0:N1], AF.Silu)
                    C.tt("dve", hTev[:, fch, 0:N0], sgt[:, 0:N0], pu0[:, 0:N0], ALU.mult)
                    C.tt("dve", hTev[:, fch, N0:CAP], sgt[:, N0:CAP], pu1[:, 0:N1], ALU.mult)
            for n in range(4):
                pacc = [banks[(bi + z) % 6] for z in range(NCH)]
                bi += NCH
                for kk in range(7):
                    wd = C.slab(io["moe_w_down"][e_, kk * 1024:(kk + 1) * 1024, n * 512:(n + 1) * 512], 8, 512)
                    for k in range(NCH):
                        for c in range(8):
                            fc = kk * 8 + c
                            C.mm(pacc[k][:, :], hTev[:, fc, k * 128:(k + 1) * 128], wd[:, c, :], fc == 0, fc == 55)
                for k in range(NCH):
                    j = e_ * NCH + k
                    C.ts("dve", oe[k][:, n * 512:(n + 1) * 512], pacc[k][:, :], gsl[:, j:j + 1], None, ALU.mult)
            for k in range(NCH):
                j = e_ * NCH + k
                o_ = oe[k]
                S.dma("pool", io["ybuf"][:, :], o_[:, :], fn=lambda e: e.indirect_dma_start(
                    out=io["ybuf"][:, :], out_offset=bass.IndirectOffsetOnAxis(ap=idxi[:, j:j + 1], axis=0),
                    in_=o_[:, :], in_offset=None, bounds_check=breg, oob_is_err=False, compute_op=ALU.add),
                    extra_reads=[idxi[:, j:j + 1]])
    C.barrier()


def phase_B_final(C, io):
    S = C.S
    es = ExitStack()
    with es:
        def sb(name, shape, dt):
            return es.enter_context(C.nc.sbuf_tensor("sbF_" + name, shape, dt))
        gbc = sb("gbc", [128, D], F32)
        bbc = sb("bbc", [128, D], F32)
        C.bcast_load(gbc[:, :], io["ln_ffn_g"])
        C.bcast_load(bbc[:, :], io["ln_ffn_b"])
        yt = [sb("yt%d" % i, [128, D], F32) for i in range(3)]
        stats = sb("stats", [128, 4, 6], F32)
        mv = sb("mv", [128, 2], F32)
        rstd = sb("rstd", [128, 1], F32)
        for q in range(16):
            y = yt[q % 3]
            S.dma("sp", y[:, :], io["ybuf"][q * 128:(q + 1) * 128, :])
            C.layernorm(y[:, :], gbc[:, :], bbc[:, :], stats, mv, rstd)
            S.dma("sp", io["out"][q * 128:(q + 1) * 128, :], y[:, :])


def _barrier(self):
    for e in self.S.E:
        self.S.wait_all(e)
    self.S.track = {}


Ctx.barrier = _barrier


def build_fused():
    nc = bass.Bass("TRN2", target_bir_lowering=False)
    es = ExitStack()
    NT = 8
    with es:
        C = Ctx(nc, es)
        io = {}
        io["xc"] = C.dram_in("xc", [2 * (128 + TPC), D])
        io["bands"] = C.dram_in("bands", [24, 128, 128])
        io["identb"] = C.dram_in("identb", [128, 128], BF16)
        io["identf"] = C.dram_in("identf", [128, 128])
        io["perm"] = C.dram_in("perm", [128, 128])
        io["tri"] = C.dram_in("tri", [128, 128], BF16)
        io["ustrict"] = C.dram_in("ustrict", [128, 128], BF16)
        io["iota"] = C.dram_in("iota", [1, CAP])
        io["tokid"] = C.dram_in("tokid", [128, 16])
        io["cosT"] = C.dram_in("cosT", [128, 2 * TPC])
        io["sinT"] = C.dram_in("sinT", [128, 2 * TPC])
        io["validb"] = C.dram_in("validb", [1, 128])
        io["valid01"] = C.dram_in("valid01", [1, 128])
        io["pool_w"] = C.dram_in("pool_w", [4, 512, 512])
        io["pool_scale"] = C.dram_in("pool_scale", [1, D])
        lnA = {k: C.dram_in(k + "0", [1, D]) for k in ("ln_mix_g", "ln_mix_b", "ln_ffn_g", "ln_ffn_b")}
        lnB = {k: C.dram_in(k + "1", [1, D]) for k in ("ln_mix_g", "ln_mix_b", "ln_ffn_g", "ln_ffn_b")}
        io["ffn_w_gate"] = C.dram_in("ffn_w_gate", [D, DFF])
        io["ffn_w_up"] = C.dram_in("ffn_w_up", [D, DFF])
        io["ffn_w_down"] = C.dram_in("ffn_w_down", [DFF, D])
        io["w_kv"] = C.dram_in("w_kv", [D, 2 * D])
        io["moba_wq"] = C.dram_in("moba_wq", [D, D])
        io["moba_wo"] = C.dram_in("moba_wo", [D, D])
        io["moe_router"] = C.dram_in("moe_router", [D, NE])
        io["moe_w_gate"] = C.dram_in("moe_w_gate", [NE, D, DFE])
        io["moe_w_up"] = C.dram_in("moe_w_up", [NE, D, DFE])
        io["moe_w_down"] = C.dram_in("moe_w_down", [NE, DFE, D])
        io["x1"] = C.dram_out("x1", [TPC, D])
        io["kT"] = C.dram_out("kT", [NH, 128, 2 * TPC], BF16)
        io["v"] = C.dram_out("v", [2 * TPC, D], BF16)
        io["x2b"] = C.dram_out("x2b", [TPC, D], BF16)
        io["ybuf"] = C.dram_out("ybuf", [TPC, D])
        io["out"] = C.dram_out("out", [TPC, D])
        C.S.dma("sp", C.identb[:, :], io["identb"])
        C.S.dma("sp", C.identf[:, :], io["identf"])
        st = {"maskf": C.sb("maskf", [128, 128], F32), "maskb": C.sb("maskb", [128, 128], BF16),
              "gate": C.sb("gate", [128, 128], F32), "kms": C.sb("kms", [128, NH * 16], F32)}
        ioA = dict(io)
        ioA.update(lnA)
        phase_A(C, NT, ioA, st["kms"])
        ioB = dict(io)
        ioB.update(lnB)
        phase_B_attn(C, ioB, st)
        phase_B_moe(C, ioB, st)
        phase_B_final(C, ioB)
        C.S.wait_all("sp")
        print("fused: ins", C.S.n_ins, "waits", C.S.n_wait)
    return nc


_CACHE = {}


def _consts():
    import ml_dtypes
    bf = ml_dtypes.bfloat16
    c = {}
    c["identb"] = np.eye(128, dtype=np.float32).astype(bf)
    c["identf"] = np.eye(128, dtype=np.float32)
    c["perm"] = perm_matrix()
    q = np.arange(128)[:, None]
    k = np.arange(128)[None, :]
    c["tri"] = np.where(k <= q, 0.0, NEGM).astype(np.float32).astype(bf)
    c["ustrict"] = (q < k).astype(np.float32).astype(bf)
    c["iota"] = np.arange(CAP, dtype=np.float32)[None, :]
    c["tokid"] = (np.arange(16)[None, :] * 128 + np.arange(128)[:, None]).astype(np.float32)
    return c


def core_inputs(c, x, shared, cst):
    f32 = np.float32
    b, hf = c // 2, c % 2
    segs = [hf * TPC, (1 - hf) * TPC]
    parts = []
    pos = []
    for t0 in segs:
        parts.append(np.zeros((128, D), f32) if t0 == 0 else x[b, t0 - 128:t0])
        parts.append(x[b, t0:t0 + TPC])
        pos.append(np.arange(t0, t0 + TPC))
    cos, sin = rope_tables_T(np.concatenate(pos))
    vb = np.zeros((8, 16), f32)
    v01 = np.zeros((8, 16), f32)
    for lb in range(8):
        for cc in range(16):
            ok = (cc < lb) if cc < 8 else (hf == 1)
            v01[lb, cc] = 1.0 if ok else 0.0
            vb[lb, cc] = 0.0 if ok else -1e30
    m = dict(shared)
    m.update(xc=np.ascontiguousarray(np.concatenate(parts, 0)), bands=make_bands([t0 == 0 for t0 in segs]),
             cosT=cos, sinT=sin, validb=vb.reshape(1, 128), valid01=v01.reshape(1, 128))
    return m


def kernel(x, pool_w, pool_scale, w_kv, moba_wq, moba_wo, ffn_w_gate, ffn_w_up, ffn_w_down,
           moe_router, moe_w_gate, moe_w_up, moe_w_down, ln_mix_g, ln_mix_b, ln_ffn_g, ln_ffn_b):
    f32 = np.float32
    A = lambda a: np.ascontiguousarray(np.asarray(a, dtype=f32))
    x = A(x)
    cst = _consts()
    if "F" not in _CACHE:
        _CACHE["F"] = build_fused()
    nc = _CACHE["F"]
    shared = dict(identb=cst["identb"], identf=cst["identf"], perm=cst["perm"], tri=cst["tri"], ustrict=cst["ustrict"],
                  iota=cst["iota"], tokid=cst["tokid"], pool_w=A(pool_w[0]), pool_scale=A(pool_scale[0:1]),
                  ffn_w_gate=A(ffn_w_gate[0]), ffn_w_up=A(ffn_w_up[0]), ffn_w_down=A(ffn_w_down[0]), w_kv=A(w_kv),
                  moba_wq=A(moba_wq[0]), moba_wo=A(moba_wo[0]), moe_router=A(moe_router[0]),
                  moe_w_gate=A(moe_w_gate[0]), moe_w_up=A(moe_w_up[0]), moe_w_down=A(moe_w_down[0]))
    for k, a in (("ln_mix_g", ln_mix_g), ("ln_mix_b", ln_mix_b), ("ln_ffn_g", ln_ffn_g), ("ln_ffn_b", ln_ffn_b)):
        shared[k + "0"] = A(a[0:1])
        shared[k + "1"] = A(a[1:2])
    in_maps = [core_inputs(c, x, shared, cst) for c in range(NCORE)]
    res = run_bass_kernel_spmd(nc, in_maps, core_ids=list(range(NCORE))).results
    out = np.stack([np.concatenate([res[2 * b]["out"], res[2 * b + 1]["out"]], 0) for b in range(NB_)], 0)
    return out.astype(f32)
```

```python
import numpy as np
from contextlib import ExitStack
import concourse.bass as bass
import concourse.mybir as mybir
from concourse.bass_utils import run_bass_kernel_spmd

F32 = mybir.dt.float32
BF16 = mybir.dt.bfloat16
I32 = mybir.dt.int32
ALU = mybir.AluOpType
AF = mybir.ActivationFunctionType
AX = mybir.AxisListType

D = 2048
SEQ = 4096
NB_ = 4
DFF = 5632
NE = 8
DFE = 7168
HD = 128
NH = 16
BLK = 256
ALPHA = float((2.0 * 2) ** 0.25)
EPS = 1e-5
WINS = (2, 4, 8, 16)
NCORE = 8
TPC = 2048
CAP = 640
NEGM = -30000.0
import os
ATT_PIPE = 2


def _box(ap):
    t = ap.tensor
    shp = list(t.shape)
    row = 1
    for s in shp[1:]:
        row *= int(s)
    off = int(ap.offset)
    dims = [(int(a), int(b)) for a, b in ap.ap]
    if "DRam" in type(t).__name__:
        lo = off
        hi = off + sum((c - 1) * abs(s) for s, c in dims) + 1
        return (t.name, 0, 1, lo, hi)
    pstep, pcnt = dims[0]
    p0 = off // row
    f0 = off % row
    if pstep == 0:
        pcnt = 1
    p1 = p0 + pcnt
    f1 = f0 + sum((c - 1) * abs(s) for s, c in dims[1:]) + 1
    return (t.name, p0, p1, f0, f1)


def _ovl(a, b):
    return a[1] < b[2] and b[1] < a[2] and a[3] < b[4] and b[3] < a[4]


def _covers(a, b):
    return a[1] <= b[1] and a[2] >= b[2] and a[3] <= b[3] and a[4] >= b[4]


class Sched:
    def __init__(self, nc, es, n_dma_slots=24):
        self.nc = nc
        self.E = {"pe": nc.tensor, "dve": nc.vector, "act": nc.scalar, "pool": nc.gpsimd, "sp": nc.sync}
        self.sem = {}
        self.cnt = {}
        for e in self.E:
            self.sem[e] = es.enter_context(nc.semaphore("s_" + e))
            self.cnt[e] = 0
        self.known = {e: {} for e in self.E}
        self.dq = {}
        for q in ("sp", "pool", "act"):
            sl = [es.enter_context(nc.semaphore("d_%s_%d" % (q, i))) for i in range(n_dma_slots)]
            self.dq[q] = {"sems": sl, "vals": [0] * n_dma_slots, "next": 0}
        self.semobj = {}
        for e in self.E:
            self.semobj[("c", e)] = self.sem[e]
        for q in self.dq:
            for i, s in enumerate(self.dq[q]["sems"]):
                self.semobj[("d", q, i)] = s
        self.track = {}
        self.n_wait = 0
        self.n_ins = 0

    def _deps(self, reads, writes):
        toks = []
        rb = [_box(a) for a in reads]
        wb = [_box(a) for a in writes]
        for b in rb:
            for ent in self.track.get(b[0], ()):
                if ent[1] == "w" and _ovl(ent[0], b):
                    toks.append(ent[2])
        for b in wb:
            for ent in self.track.get(b[0], ()):
                if _ovl(ent[0], b):
                    toks.append(ent[2])
        return toks, rb, wb

    def _record(self, rb, wb, tok):
        for b in wb:
            lst = self.track.setdefault(b[0], [])
            lst[:] = [e for e in lst if not _covers(b, e[0])]
            lst.append([b, "w", tok])
        for b in rb:
            lst = self.track.setdefault(b[0], [])
            lst[:] = [e for e in lst if not (e[1] == "r" and e[2][0] == tok[0] and _covers(b, e[0]))]
            lst.append([b, "r", tok])

    def _emit_waits(self, eng, toks):
        need = {}
        for k, v in toks:
            if v > need.get(k, 0):
                need[k] = v
        kn = self.known[eng]
        for k, v in need.items():
            if kn.get(k, 0) >= v:
                continue
            self.E[eng].wait_ge(self.semobj[k], v)
            self.n_wait += 1
            kn[k] = v

    def op(self, eng, fn, reads=(), writes=(), same_ok=False):
        toks, rb, wb = self._deps(reads, writes)
        if same_ok:
            toks = [t for t in toks if t[0] != ("c", eng)]
        self._emit_waits(eng, toks)
        ins = fn(self.E[eng])
        self.cnt[eng] += 1
        ins.then_inc(self.sem[eng], 1)
        self._record(rb, wb, (("c", eng), self.cnt[eng]))
        self.n_ins += 1
        return ins

    def dma(self, q, out, in_, fn=None, extra_reads=()):
        toks, rb, wb = self._deps([in_] + list(extra_reads), [out])
        d = self.dq[q]
        i = d["next"]
        d["next"] = (i + 1) % len(d["sems"])
        key = ("d", q, i)
        if d["vals"][i] > 0:
            toks.append((key, d["vals"][i]))
        self._emit_waits(q, toks)
        if fn is None:
            ins = self.E[q].dma_start(out=out, in_=in_)
        else:
            ins = fn(self.E[q])
        d["vals"][i] += 16
        ins.then_inc(d["sems"][i], 16)
        self._record(rb, wb, (key, d["vals"][i]))
        self.n_ins += 1

    def wait_all(self, eng):
        toks = []
        for e in self.E:
            if self.cnt[e] > 0:
                toks.append((("c", e), self.cnt[e]))
        for q, d in self.dq.items():
            for i, v in enumerate(d["vals"]):
                if v > 0:
                    toks.append((("d", q, i), v))
        self._emit_waits(eng, toks)


class Ctx:
    def __init__(self, nc, es, nring=5):
        self.nc = nc
        self.es = es
        self.S = Sched(nc, es)
        self.ring = [es.enter_context(nc.sbuf_tensor("ring%d" % i, [128, 4096], BF16)) for i in range(nring)]
        self.ring_i = 0
        self.psA = [es.enter_context(nc.psum_tensor("psA%d" % i, [128, 512], F32)) for i in range(4)]
        self.psT = [es.enter_context(nc.psum_tensor("psT%d" % i, [128, 1024], BF16)) for i in range(2)]
        self.psB = [es.enter_context(nc.psum_tensor("psB%d" % i, [128, 512], F32)) for i in range(2)]
        self.iA = 0
        self.iT = 0
        self.iB = 0
        self.identb = self.sb("identb", [128, 128], BF16)
        self.identf = self.sb("identf", [128, 128], F32)

    def sb(self, name, shape, dt):
        return self.es.enter_context(self.nc.sbuf_tensor("sb_" + name, shape, dt))

    def dram_in(self, name, shape, dt=F32):
        return self.nc.dram_tensor(name, list(shape), dt, kind="ExternalInput").ap()

    def dram_out(self, name, shape, dt=F32):
        return self.nc.dram_tensor(name, list(shape), dt, kind="ExternalOutput").ap()

    def nextA(self):
        p = self.psA[self.iA % 4]
        self.iA += 1
        return p

    def nextT(self):
        p = self.psT[self.iT % 2]
        self.iT += 1
        return p

    def nextB(self):
        p = self.psB[self.iB % 2]
        self.iB += 1
        return p

    def mm(self, out, lhsT, rhs, start, stop):
        self.S.op("pe", lambda e: e.matmul(out, lhsT, rhs, start=start, stop=stop),
                  reads=[lhsT, rhs], writes=[out], same_ok=True)

    def tr(self, out, in_, ident):
        self.S.op("pe", lambda e: e.transpose(out, in_, ident), reads=[in_, ident], writes=[out], same_ok=True)

    def tt(self, eng, out, a, b, op):
        self.S.op(eng, lambda e: e.tensor_tensor(out, a, b, op), reads=[a, b], writes=[out])

    def ts(self, eng, out, a, s1, s2, op0, op1=None):
        rd = [a] + [s for s in (s1, s2) if not isinstance(s, (int, float)) and s is not None]
        if op1 is None:
            self.S.op(eng, lambda e: e.tensor_scalar(out, a, s1, None, op0), reads=rd, writes=[out])
        else:
            self.S.op(eng, lambda e: e.tensor_scalar(out, a, s1, s2, op0, op1), reads=rd, writes=[out])

    def stt(self, eng, out, a, sc, b, op0, op1):
        rd = [a, b] + ([] if isinstance(sc, (int, float)) else [sc])
        self.S.op(eng, lambda e: e.scalar_tensor_tensor(out, a, sc, b, op0, op1), reads=rd, writes=[out])

    def cp(self, eng, out, a):
        if eng == "act":
            self.S.op("act", lambda e: e.activation(out, a, AF.Copy), reads=[a], writes=[out])
        else:
            self.S.op(eng, lambda e: e.tensor_copy(out, a), reads=[a], writes=[out])

    def act(self, out, a, func, bias=None, scale=None, accum_out=None):
        kw = {}
        rd = [a]
        wr = [out]
        if bias is not None:
            kw["bias"] = bias
            if not isinstance(bias, (int, float)):
                rd.append(bias)
        if scale is not None:
            kw["scale"] = scale
            if not isinstance(scale, (int, float)):
                rd.append(scale)
        if accum_out is not None:
            kw["accum_out"] = accum_out
            wr.append(accum_out)
        self.S.op("act", lambda e: e.activation(out, a, func, **kw), reads=rd, writes=wr)

    def slab(self, w2d, kc, ncols):
        r = self.ring[self.ring_i % len(self.ring)]
        self.ring_i += 1
        v = r[:, 0:kc * ncols].rearrange("p (c n) -> p c n", n=ncols)
        self.S.dma("pool", v, w2d.rearrange("(c p) n -> p c n", p=128))
        return v

    def bcast_load(self, dst, row_ap, q="sp"):
        self.S.dma(q, dst, row_ap.broadcast_to([128, int(row_ap.shape[-1])]))

    def layernorm(self, h, gbc, bbc, stats, mv, rstd):
        S = self.S
        for k in range(4):
            S.op("dve", lambda e: e.bn_stats(stats[:, k, :], h[:, k * 512:(k + 1) * 512]),
                 reads=[h[:, k * 512:(k + 1) * 512]], writes=[stats[:, k, :]])
        S.op("dve", lambda e: e.bn_aggr(mv[:, :], stats[:, :, :]), reads=[stats[:, :, :]], writes=[mv[:, :]])
        self.ts("dve", rstd[:, :], mv[:, 1:2], EPS, None, ALU.add)
        self.act(rstd[:, :], rstd[:, :], AF.Sqrt)
        self.S.op("dve", lambda e: e.reciprocal(rstd[:, :], rstd[:, :]), reads=[rstd[:, :]], writes=[rstd[:, :]])
        self.stt("dve", h, h, mv[:, 0:1], gbc, ALU.subtract, ALU.mult)
        self.stt("dve", h, h, rstd[:, 0:1], bbc, ALU.mult, ALU.add)

    def to_feature_major(self, xbf, xT, col0):
        for c0 in range(0, 16, 4):
            pt = self.nextT()
            for j in range(4):
                c = c0 + j
                self.tr(pt[:, j * 128:(j + 1) * 128], xbf[:, c * 128:(c + 1) * 128], self.identb[:, :])
            src = pt[:, 0:512].rearrange("p (c t) -> p c t", t=128)
            self.cp("act", xT[:, c0:c0 + 4, col0:col0 + 128], src)


def phase_A(C, NT, io, kms):
    S = C.S
    es = ExitStack()
    with es:
        _phase_A_body(C, NT, io, kms, es)
    C.barrier()


def _phase_A_body(C, NT, io, kms, es):
    S = C.S

    def sb(name, shape, dt):
        return es.enter_context(C.nc.sbuf_tensor("sbA_" + name, shape, dt))
    x_in = io["xc"]
    bands = sb("bands", [128, 3 * 4 * 2 * 128], F32)
    S.dma("sp", bands[:, :].rearrange("p (a t) -> p a t", t=128), io["bands"].rearrange("a p t -> p a t"))
    perm = sb("perm", [128, 128], F32)
    S.dma("sp", perm[:, :], io["perm"])
    poolw = sb("poolw", [128, 4 * 4 * 512], BF16)
    for g in range(4):
        S.dma("pool", poolw[:, g * 2048:(g + 1) * 2048].rearrange("p (c n) -> p c n", n=512),
              io["pool_w"][g].rearrange("(c p) n -> p c n", p=128))
    pw = poolw[:, :].rearrange("p (g c n) -> p g c n", g=4, c=4)
    bd = bands[:, :].rearrange("p (f g h t) -> p f g h t", f=3, g=4, h=2)
    gbc = sb("gbc", [128, D], F32)
    bbc = sb("bbc", [128, D], F32)
    sbc = sb("sbc", [128, D], F32)
    C.bcast_load(sbc[:, :], io["pool_scale"][0:1, :])
    xb = [sb("xb%d" % i, [128, D], F32) for i in range(2)]
    xm = [sb("xm%d" % i, [128, D], F32) for i in range(4)]
    xbf2 = [sb("xbf%d" % i, [128, D], BF16) for i in range(2)]
    xT = sb("xT", [128, 16 * 512], BF16)
    xTv = xT[:, :].rearrange("p (c t) -> p c t", t=512)
    hT = sb("hT", [128, 22 * 512], BF16)
    hTv = hT[:, :].rearrange("p (c t) -> p c t", t=512)
    dT = sb("dT", [128, 4 * 128], BF16)
    dTv = dT[:, :].rearrange("p (c t) -> p c t", t=128)
    stats = sb("stats", [128, 4, 6], F32)
    mv = sb("mv", [128, 2], F32)
    rstd = sb("rstd", [128, 1], F32)
    t32 = [sb("t32_%d" % i, [128, 512], F32) for i in range(2)]
    sg = t32
    kr = [sb("kr%d" % i, [128, 512], F32) for i in range(2)]
    krb = [sb("krb%d" % i, [128, 512], BF16) for i in range(2)]
    cosb = sb("cosb", [128, 512], F32)
    sinb = sb("sinb", [128, 512], F32)
    vb = [sb("vb%d" % i, [128, 512], BF16) for i in range(2)]
    kmv = kms[:, :].rearrange("p (h b) -> p h b", h=NH)

    for ti in range(NT):
        C.bcast_load(gbc[:, :], io["ln_mix_g"][0:1, :])
        C.bcast_load(bbc[:, :], io["ln_mix_b"][0:1, :])
        for s in range(4):
            q = ti * 4 + s
            seg = q // 16
            xc, xp = xb[q % 2], xb[(q - 1) % 2]
            r0 = 128 * (seg + 1) + q * 128
            if q % 16 == 0:
                S.dma("sp", xp[:, :], x_in[r0 - 128:r0, :])
            S.dma("sp", xc[:, :], x_in[r0:r0 + 128, :])
            f = (0 if seg == 0 else 2) if q % 16 == 0 else 1
            h = xm[s]
            for g in range(4):
                pd = C.nextA()
                for cc in range(4):
                    col = g * 512 + cc * 128
                    o = pd[:, cc * 128:(cc + 1) * 128]
                    C.mm(o, xp[:, col:col + 128], bd[:, f, g, 0, :], True, False)
                    C.mm(o, xc[:, col:col + 128], bd[:, f, g, 1, :], False, True)
                C.cp("act", dTv[:, :, :], pd[:, :].rearrange("p (c t) -> p c t", t=128))
                py = C.nextA()
                for cc in range(4):
                    C.mm(py[:, :], dTv[:, cc, :], pw[:, g, cc, :], cc == 0, cc == 3)
                hs = h[:, g * 512:(g + 1) * 512]
                C.tt("dve", hs, py[:, :], sbc[:, g * 512:(g + 1) * 512], ALU.mult)
                C.stt("dve", hs, xc[:, g * 512:(g + 1) * 512], ALPHA, hs, ALU.mult, ALU.add)
            C.layernorm(h[:, :], gbc[:, :], bbc[:, :], stats, mv, rstd)
            C.cp("act", xbf2[s % 2][:, :], h[:, :])
            if s > 0:
                C.to_feature_major(xbf2[(s - 1) % 2], xTv, (s - 1) * 128)
        C.to_feature_major(xbf2[1], xTv, 3 * 128)
        for half in range(2):
            for j in range(11):
                f0 = half * 2816 + j * 256
                wg = C.slab(io["ffn_w_gate"][:, f0:f0 + 256], 16, 256)
                wu = C.slab(io["ffn_w_up"][:, f0:f0 + 256], 16, 256)
                for m in range(2):
                    pg = C.nextA()
                    pu = C.nextA()
                    for c in range(16):
                        C.mm(pg[:, :], wg[:, c, m * 128:(m + 1) * 128], xTv[:, c, :], c == 0, c == 15)
                    for c in range(16):
                        C.mm(pu[:, :], wu[:, c, m * 128:(m + 1) * 128], xTv[:, c, :], c == 0, c == 15)
                    sgt = sg[(j * 2 + m) % 2]
                    C.act(sgt[:, :], pg[:, :], AF.Silu)
                    C.tt("dve", hTv[:, j * 2 + m, :], sgt[:, :], pu[:, :], ALU.mult)
            for n in range(4):
                pacc = [C.nextA() for _ in range(4)]
                for k in range(3):
                    kc = 8 if k < 2 else 6
                    r0 = half * 2816 + k * 1024
                    wd = C.slab(io["ffn_w_down"][r0:r0 + kc * 128, n * 512:(n + 1) * 512], kc, 512)
                    for s in range(4):
                        for c in range(kc):
                            fc = k * 8 + c
                            C.mm(pacc[s][:, :], hTv[:, fc, s * 128:(s + 1) * 128], wd[:, c, :], fc == 0, fc == 21)
                for s in range(4):
                    o = xm[s][:, n * 512:(n + 1) * 512]
                    if half == 0:
                        C.stt("dve", o, o, ALPHA, pacc[s][:, :], ALU.mult, ALU.add)
                    else:
                        C.tt("dve", o, o, pacc[s][:, :], ALU.add)
        C.bcast_load(gbc[:, :], io["ln_ffn_g"][0:1, :])
        C.bcast_load(bbc[:, :], io["ln_ffn_b"][0:1, :])
        for s in range(4):
            q = ti * 4 + s
            C.layernorm(xm[s][:, :], gbc[:, :], bbc[:, :], stats, mv, rstd)
            if q < 16:
                S.dma("sp", io["x1"][q * 128:(q + 1) * 128, :], xm[s][:, :])
            C.cp("act", xbf2[s % 2][:, :], xm[s][:, :])
            if s > 0:
                C.to_feature_major(xbf2[(s - 1) % 2], xTv, (s - 1) * 128)
        C.to_feature_major(xbf2[1], xTv, 3 * 128)
        S.dma("sp", cosb[:, :], io["cosT"][:, ti * 512:(ti + 1) * 512])
        S.dma("sp", sinb[:, :], io["sinT"][:, ti * 512:(ti + 1) * 512])
        wk_hold = [None]

        def kproj(hh):
            if hh % 2 == 0:
                wk_hold[0] = C.slab(io["w_kv"][:, hh * 128:hh * 128 + 256], 16, 256)
            wk = wk_hold[0]
            m = hh % 2
            pk = C.nextA()
            for c in range(16):
                C.mm(pk[:, :], wk[:, c, m * 128:(m + 1) * 128], xTv[:, c, :], c == 0, c == 15)
            return pk

        pend = kproj(0)
        for hh in range(NH):
            pk = pend
            if hh + 1 < NH:
                pend = kproj(hh + 1)
            t = t32[hh % 2]
            C.cp("act", t[:, :], pk[:, :])
            pr = C.nextA()
            C.mm(pr[:, :], perm[:, :], t[:, :], True, True)
            k_ = kr[hh % 2]
            C.tt("dve", k_[:, :], pr[:, :], sinb[:, :], ALU.mult)
            C.tt("dve", t[:, :], t[:, :], cosb[:, :], ALU.mult)
            C.tt("dve", k_[:, :], k_[:, :], t[:, :], ALU.add)
            kb = krb[hh % 2]
            C.cp("act", kb[:, :], k_[:, :])
            S.dma("sp", io["kT"][hh, :, ti * 512:(ti + 1) * 512], kb[:, :])
            S.op("dve", lambda e: e.tensor_reduce(kmv[:, hh, ti * 2:ti * 2 + 2],
                                                   k_[:, :].rearrange("p (b t) -> p b t", t=256), AX.X, ALU.add),
                 reads=[k_[:, :]], writes=[kmv[:, hh, ti * 2:ti * 2 + 2]])
        for n in range(4):
            pacc = [C.nextA() for _ in range(4)]
            for k in range(2):
                wv = C.slab(io["w_kv"][k * 1024:(k + 1) * 1024, 2048 + n * 512:2048 + (n + 1) * 512], 8, 512)
                for s in range(4):
                    for c in range(8):
                        cc = k * 8 + c
                        C.mm(pacc[s][:, :], xTv[:, cc, s * 128:(s + 1) * 128], wv[:, c, :], cc == 0, cc == 15)
            for s in range(4):
                v_ = vb[(n * 4 + s) % 2]
                C.cp("act", v_[:, :], pacc[s][:, :])
                q = ti * 4 + s
                S.dma("sp", io["v"][q * 128:(q + 1) * 128, n * 512:(n + 1) * 512], v_[:, :])
    C.ts("dve", kms[:, :], kms[:, :], 1.0 / 256.0, None, ALU.mult)


def make_bands(seg_start):
    out = np.zeros((3, 4, 2, 128, 128), np.float32)
    for f in range(3):
        start = (f == 0 and seg_start[0]) or (f == 2 and seg_start[1])
        for g, w in enumerate(WINS):
            for t in range(128):
                cnt = min(t + 1, w) if start else w
                for k in range(w):
                    tp = t - k
                    if tp >= 0:
                        out[f, g, 1, tp, t] += 1.0 / cnt
                    elif not start:
                        out[f, g, 0, 128 + tp, t] += 1.0 / cnt
                out[f, g, 1, t, t] -= 1.0
    return out.reshape(24, 128, 128)


def rope_tables_T(pos):
    inv = 1.0 / (10000.0 ** (np.arange(0, HD, 2, dtype=np.float32) / HD))
    ang = pos.astype(np.float32)[:, None] * inv[None, :].astype(np.float32)
    ang = np.concatenate([ang, ang], axis=-1)
    cos = np.cos(ang).astype(np.float32).T.copy()
    sin = np.sin(ang).astype(np.float32).T.copy()
    sin[:64, :] *= -1.0
    return cos, sin


def perm_matrix():
    P = np.zeros((128, 128), np.float32)
    for m in range(128):
        P[(m + 64) % 128, m] = 1.0
    return P


SCALE = float(HD ** -0.5)


def phase_B_attn(C, io, st):
    S = C.S
    es = ExitStack()
    with es:
        def sb(name, shape, dt):
            return es.enter_context(C.nc.sbuf_tensor("sbB_" + name, shape, dt))
        perm = sb("perm", [128, 128], F32)
        S.dma("sp", perm[:, :], io["perm"])
        tri = sb("tri", [128, 128], BF16)
        S.dma("sp", tri[:, :], io["tri"])
        kmb = sb("kmb", [128, NH * 16], BF16)
        C.cp("dve", kmb[:, :], st["kms"][:, :])
        kmbv = kmb[:, :].rearrange("p (h b) -> p h b", h=NH)
        vbias = sb("vbias", [128, 128], F32)
        v01 = sb("v01", [128, 128], F32)
        C.bcast_load(vbias[:, :], io["validb"])
        C.bcast_load(v01[:, :], io["valid01"])
        wr = sb("wr", [128, 16 * 8], F32)
        S.dma("sp", wr[:, :].rearrange("p (c e) -> p c e", e=8), io["moe_router"].rearrange("(c p) e -> p c e", p=128))
        wrv = wr[:, :].rearrange("p (c e) -> p c e", e=8)
        gbc = sb("gbc", [128, D], F32)
        bbc = sb("bbc", [128, D], F32)
        C.bcast_load(gbc[:, :], io["ln_mix_g"])
        C.bcast_load(bbc[:, :], io["ln_mix_b"])
        xm = [sb("xm%d" % i, [128, D], F32) for i in range(4)]
        xbf = sb("xbf", [128, D], BF16)
        ytmp = sb("ytmp", [128, D], F32)
        xT = sb("xT", [128, 16 * 512], BF16)
        xTv = xT[:, :].rearrange("p (c t) -> p c t", t=512)
        attn = [sb("attn%d" % i, [128, D], BF16) for i in range(4)]
        kbuf = [sb("kbuf%d" % i, [128, 15 * 256], BF16) for i in range(2)]
        vbuf = [sb("vbuf%d" % i, [128, 30 * 128], BF16) for i in range(2)]
        kob = [sb("kob%d" % i, [128, 512], BF16) for i in range(2)]
        vob = [sb("vob%d" % i, [128, 4 * 128], BF16) for i in range(2)]
        cosb = sb("cosb", [128, 512], F32)
        sinb = sb("sinb", [128, 512], F32)
        t32 = [sb("t32_%d" % i, [128, 512], F32) for i in range(2)]
        qf = [sb("qf%d" % i, [128, 512], F32) for i in range(2)]
        qb = [sb("qb%d" % i, [128, 512], BF16) for i in range(2)]
        gsb = [sb("gsb%d" % i, [128, 16], F32) for i in range(8)]
        m8 = [sb("m8_%d" % i, [128, 8], F32) for i in range(8)]
        selt = [sb("sel%d" % i, [128, 16], F32) for i in range(8)]
        biast = [sb("bias%d" % i, [128, 16], F32) for i in range(8)]
        rs = [sb("rs%d" % i, [128, 16], F32) for i in range(8)]
        tot = [sb("tot%d" % i, [128, 1], F32) for i in range(8)]
        pbuf = [sb("pbuf%d" % i, [128, 256], BF16) for i in range(4)]
        ptb = [sb("ptb%d" % i, [128, 256], BF16) for i in range(4)]
        stats = sb("stats", [128, 4, 6], F32)
        mv = sb("mv", [128, 2], F32)
        rstd = sb("rstd", [128, 1], F32)
        x2Tf = sb("x2Tf", [128, 16 * 128], F32)
        x2Tv = x2Tf[:, :].rearrange("p (c t) -> p c t", t=128)
        lg = sb("lg", [128, 8], F32)
        rm8 = sb("rm8", [128, 8], F32)
        rex = sb("rex", [128, 8], F32)
        rneg = sb("rneg", [128, 1], F32)
        rden = sb("rden", [128, 1], F32)
        kp = 0
        it = 0
        for gi in range(4):
            for s in range(4):
                q = gi * 4 + s
                S.dma("sp", xm[s][:, :], io["x1"][q * 128:(q + 1) * 128, :])
                C.cp("act", xbf[:, :], xm[s][:, :])
                C.to_feature_major(xbf, xTv, s * 128)
            S.dma("sp", cosb[:, :], io["cosT"][:, gi * 512:(gi + 1) * 512])
            S.dma("sp", sinb[:, :], io["sinT"][:, gi * 512:(gi + 1) * 512])
            n0 = 2 * gi + 1
            cands = list(range(0, n0)) + list(range(8, 16))
            ncand = len(cands)
            wq_hold = [None]

            def pre(h):
                m = h % 2
                if m == 0:
                    wq_hold[0] = C.slab(io["moba_wq"][:, h * 128:h * 128 + 256], 16, 256)
                wq = wq_hold[0]
                kh = kbuf[m]
                vh = vbuf[m][:, :].rearrange("p (c d) -> p c d", d=128)
                ko = kob[m]
                vo = vob[m][:, :].rearrange("p (c d) -> p c d", d=128)
                S.dma("sp", kh[:, 0:n0 * 256], io["kT"][h, :, 0:n0 * 256])
                S.dma("sp", kh[:, n0 * 256:ncand * 256], io["kT"][h, :, TPC:2 * TPC])
                S.dma("sp", vh[:, 0:n0 * 2, :],
                      io["v"][0:n0 * 256, h * 128:(h + 1) * 128].rearrange("(c p) d -> p c d", p=128))
                S.dma("sp", vh[:, n0 * 2:ncand * 2, :],
                      io["v"][TPC:2 * TPC, h * 128:(h + 1) * 128].rearrange("(c p) d -> p c d", p=128))
                S.dma("sp", ko[:, :], io["kT"][h, :, gi * 512:(gi + 1) * 512])
                S.dma("sp", vo[:, :, :],
                      io["v"][gi * 512:(gi + 1) * 512, h * 128:(h + 1) * 128].rearrange("(c p) d -> p c d", p=128))
                pq = C.nextA()
                for c in range(16):
                    C.mm(pq[:, :], wq[:, c, m * 128:(m + 1) * 128], xTv[:, c, :], c == 0, c == 15)
                t = t32[m]
                C.cp("act", t[:, :], pq[:, :])
                pr = C.nextA()
                C.mm(pr[:, :], perm[:, :], t[:, :], True, True)
                q_ = qf[m]
                C.tt("dve", q_[:, :], pr[:, :], sinb[:, :], ALU.mult)
                C.tt("dve", t[:, :], t[:, :], cosb[:, :], ALU.mult)
                qbt = qb[m]
                C.tt("dve", qbt[:, :], q_[:, :], t[:, :], ALU.add)
                pgt = C.nextA()
                for r in range(4):
                    C.mm(pgt[:, r * 16:(r + 1) * 16], qbt[:, r * 128:(r + 1) * 128], kmbv[:, h, :], True, True)
                for r in range(4):
                    lb = gi * 2 + r // 2
                    k8 = m * 4 + r
                    g_, m8_, sl_, bi_ = gsb[k8], m8[k8], selt[k8], biast[k8]
                    C.tt("dve", g_[:, :], pgt[:, r * 16:(r + 1) * 16], vbias[:, lb * 16:(lb + 1) * 16], ALU.add)
                    S.op("dve", lambda e: e.max(m8_[:, :], g_[:, :]), reads=[g_[:, :]], writes=[m8_[:, :]])
                    C.ts("dve", sl_[:, :], g_[:, :], m8_[:, 2:3], None, ALU.is_ge)
                    C.tt("dve", sl_[:, :], sl_[:, :], v01[:, lb * 16:(lb + 1) * 16], ALU.mult)
                    C.ts("dve", bi_[:, :], sl_[:, :], 1.0, -NEGM, ALU.subtract, ALU.mult)

            def stage_a(w):
                h, r, m = w["h"], w["r"], w["h"] % 2
                qt = qb[m][:, r * 128:(r + 1) * 128]
                k8 = m * 4 + r
                ps = C.nextA()
                p = pbuf[w["i"] % 4]
                w["p"] = p
                if w["kind"] == "past":
                    cs, c = w["cs"], w["c"]
                    C.mm(ps[:, 0:256], qt, kbuf[m][:, cs * 256:(cs + 1) * 256], True, True)
                    C.act(p[:, :], ps[:, 0:256], AF.Exp, bias=biast[k8][:, c:c + 1], scale=SCALE,
                          accum_out=rs[k8][:, cs:cs + 1])
                else:
                    rr = r % 2
                    nk = rr + 1
                    boff = (r // 2) * 256
                    ko = kob[m]
                    for kc in range(nk):
                        o = ps[:, kc * 128:(kc + 1) * 128]
                        if kc == rr:
                            C.mm(o, qt, ko[:, boff + kc * 128:boff + (kc + 1) * 128], True, False)
                            C.mm(o, C.identb[:, :], tri[:, :], False, True)
                        else:
                            C.mm(o, qt, ko[:, boff + kc * 128:boff + (kc + 1) * 128], True, True)
                    C.act(p[:, 0:nk * 128], ps[:, 0:nk * 128], AF.Exp, scale=SCALE, accum_out=rs[k8][:, ncand:ncand + 1])

            def stage_b(w):
                nk = 2 if w["kind"] == "past" else (w["r"] % 2 + 1)
                i = w["i"]
                pt = C.psT[i % 2][:, ((i // 2) % 4) * 256:((i // 2) % 4) * 256 + 256]
                p = w["p"]
                for kc in range(nk):
                    C.tr(pt[:, kc * 128:(kc + 1) * 128], p[:, kc * 128:(kc + 1) * 128], C.identb[:, :])
                pts = ptb[i % 4]
                C.cp("dve", pts[:, 0:nk * 128], pt[:, 0:nk * 128])
                w["pts"] = pts

            def stage_c(w):
                h, r, m = w["h"], w["r"], w["h"] % 2
                k8 = m * 4 + r
                po = C.psB[(h * 4 + r) % 2]
                pts = w["pts"]
                if w["kind"] == "past":
                    vh = vbuf[m][:, :].rearrange("p (c d) -> p c d", d=128)
                    for kc in range(2):
                        C.mm(po[:, 0:128], pts[:, kc * 128:(kc + 1) * 128], vh[:, w["cs"] * 2 + kc, :],
                             w["cs"] == 0 and kc == 0, False)
                else:
                    nk = r % 2 + 1
                    vo = vob[m][:, :].rearrange("p (c d) -> p c d", d=128)
                    for kc in range(nk):
                        C.mm(po[:, 0:128], pts[:, kc * 128:(kc + 1) * 128], vo[:, (r // 2) * 2 + kc, :], False, kc == nk - 1)
                    tot_ = tot[k8]
                    S.op("dve", lambda e: e.tensor_reduce(tot_[:, :], rs[k8][:, 0:ncand + 1], AX.X, ALU.add),
                         reads=[rs[k8][:, 0:ncand + 1]], writes=[tot_[:, :]])
                    S.op("dve", lambda e: e.reciprocal(tot_[:, :], tot_[:, :]), reads=[tot_[:, :]], writes=[tot_[:, :]])
                    C.ts("dve", attn[r][:, h * 128:(h + 1) * 128], po[:, 0:128], tot_[:, 0:1], None, ALU.mult)

            pre(0)
            pend_b = None
            pend_c = None
            for h in range(NH):
                items = []
                for r in range(4):
                    for cs, c in enumerate(cands):
                        items.append(dict(h=h, r=r, kind="past", cs=cs, c=c))
                    items.append(dict(h=h, r=r, kind="own"))
                for wi, w in enumerate(items):
                    if wi == 3 and h + 1 < NH:
                        pre(h + 1)
                    w["i"] = kp
                    kp += 1
                    if ATT_PIPE == 0:
                        stage_a(w)
                        stage_b(w)
                        stage_c(w)
                        continue
                    stage_a(w)
                    if ATT_PIPE == 1:
                        if pend_b is not None:
                            stage_b(pend_b)
                            stage_c(pend_b)
                        pend_b = w
                        continue
                    if pend_b is not None:
                        stage_b(pend_b)
                    if pend_c is not None:
                        stage_c(pend_c)
                    pend_c = pend_b
                    pend_b = w
            if pend_b is not None:
                stage_b(pend_b)
                if pend_c is not None:
                    stage_c(pend_c)
                stage_c(pend_b)
            pend_b = None
            pend_c = None
            for s in range(4):
                C.to_feature_major(attn[s], xTv, s * 128)
            for n in range(4):
                pacc = [C.nextA() for _ in range(4)]
                for k in range(2):
                    wo = C.slab(io["moba_wo"][k * 1024:(k + 1) * 1024, n * 512:(n + 1) * 512], 8, 512)
                    for s in range(4):
                        for c in range(8):
                            hh = k * 8 + c
                            C.mm(pacc[s][:, :], xTv[:, hh, s * 128:(s + 1) * 128], wo[:, c, :], hh == 0, hh == 15)
                for s in range(4):
                    o = xm[s][:, n * 512:(n + 1) * 512]
                    C.stt("dve", o, o, ALPHA, pacc[s][:, :], ALU.mult, ALU.add)
            for s in range(4):
                q = gi * 4 + s
                C.layernorm(xm[s][:, :], gbc[:, :], bbc[:, :], stats, mv, rstd)
                C.cp("act", xbf[:, :], xm[s][:, :])
                S.dma("sp", io["x2b"][q * 128:(q + 1) * 128, :], xbf[:, :])
                C.ts("dve", ytmp[:, :], xm[s][:, :], ALPHA, None, ALU.mult)
                S.dma("sp", io["ybuf"][q * 128:(q + 1) * 128, :], ytmp[:, :])
                for c0 in range(0, 16, 4):
                    pt = C.nextA()
                    for j in range(4):
                        c = c0 + j
                        C.tr(pt[:, j * 128:(j + 1) * 128], xm[s][:, c * 128:(c + 1) * 128], C.identf[:, :])
                    C.cp("act", x2Tv[:, c0:c0 + 4, :], pt[:, :].rearrange("p (c t) -> p c t", t=128))
                pl = C.nextA()
                for c in range(16):
                    C.mm(pl[:, 0:8], x2Tv[:, c, :], wrv[:, c, :], c == 0, c == 15)
                C.cp("dve", lg[:, :], pl[:, 0:8])
                S.op("dve", lambda e: e.max(rm8[:, :], lg[:, :]), reads=[lg[:, :]], writes=[rm8[:, :]])
                mf = st["maskf"][:, q * 8:(q + 1) * 8]
                C.ts("dve", mf, lg[:, :], rm8[:, 1:2], None, ALU.is_ge)
                C.cp("dve", st["maskb"][:, q * 8:(q + 1) * 8], mf)
                C.ts("dve", rneg[:, :], rm8[:, 0:1], -1.0, None, ALU.mult)
                C.act(rex[:, :], lg[:, :], AF.Exp, bias=rneg[:, 0:1], scale=1.0)
                C.tt("dve", rex[:, :], rex[:, :], mf, ALU.mult)
                S.op("dve", lambda e: e.tensor_reduce(rden[:, :], rex[:, :], AX.X, ALU.add), reads=[rex[:, :]], writes=[rden[:, :]])
                S.op("dve", lambda e: e.reciprocal(rden[:, :], rden[:, :]), reads=[rden[:, :]], writes=[rden[:, :]])
                C.ts("dve", st["gate"][:, q * 8:(q + 1) * 8], rex[:, :], rden[:, 0:1], None, ALU.mult)
    C.barrier()


def phase_B_moe(C, io, st):
    S = C.S
    es = ExitStack()
    NCH = CAP // 128
    with es:
        def sb(name, shape, dt):
            return es.enter_context(C.nc.sbuf_tensor("sbM_" + name, shape, dt))
        onesb = sb("onesb", [128, 128], BF16)
        ustr = sb("ustr", [128, 128], BF16)
        S.op("dve", lambda e: e.memset(onesb[:, :], 1.0), writes=[onesb[:, :]])
        S.dma("sp", ustr[:, :], io["ustrict"])
        iot = sb("iot", [128, CAP], F32)
        C.bcast_load(iot[:, :], io["iota"])
        tokf = sb("tokf", [128, 16], F32)
        S.dma("sp", tokf[:, :], io["tokid"])
        slotm = sb("slotm", [128, 16 * 8], F32)
        tmp8 = sb("tmp8", [128, 8], F32)
        tg = sb("tg", [128, 16 * 8 * 3], F32)
        tgv = tg[:, :].rearrange("p (i e k) -> p i e k", i=16, e=8)
        maskb = st["maskb"][:, :].rearrange("p (i e) -> p i e", e=8)
        maskf = st["maskf"][:, :].rearrange("p (i e) -> p i e", e=8)
        gatev = st["gate"][:, :].rearrange("p (i e) -> p i e", e=8)
        for i in range(16):
            pr = C.nextA()
            for i2 in range(i):
                C.mm(pr[:, 0:8], onesb[:, :], maskb[:, i2, :], i2 == 0, False)
            C.mm(pr[:, 0:8], ustr[:, :], maskb[:, i, :], i == 0, True)
            C.stt("dve", tmp8[:, :], pr[:, 0:8], 1.0, maskf[:, i, :], ALU.add, ALU.mult)
            C.ts("dve", slotm[:, i * 8:(i + 1) * 8], tmp8[:, :], -1.0, None, ALU.add)
        for e_ in range(8):
            C.cp("dve", tgv[:, :, e_, 0], tokf[:, :])
        C.cp("dve", tgv[:, :, :, 1], gatev)
        S.op("dve", lambda e: e.memset(tgv[:, :, :, 2], 1.0), writes=[tgv[:, :, :, 2]])
        selm = [sb("selm%d" % i, [128, CAP], F32) for i in range(3)]
        tab = sb("tab", [128, 8 * NCH * 4], F32)
        tabv = tab[:, :].rearrange("p (e k c) -> p e k c", e=8, k=NCH)
        idxf = sb("idxf", [128, 8 * NCH], F32)
        idxi = sb("idxi", [128, 8 * NCH], I32)
        gsl = sb("gsl", [128, 8 * NCH], F32)
        k3 = 0
        for e_ in range(8):
            ptab = (C.psA + C.psB)[0:NCH]
            for i in range(16):
                sm = selm[k3 % 3]
                k3 += 1
                C.ts("dve", sm[:, :], iot[:, :], slotm[:, i * 8 + e_:i * 8 + e_ + 1], None, ALU.is_equal)
                for k in range(NCH):
                    C.mm(ptab[k][:, 0:3], sm[:, k * 128:(k + 1) * 128], tgv[:, i, e_, :], i == 0, i == 15)
            for k in range(NCH):
                C.cp("dve", tabv[:, e_, k, 0:3], ptab[k][:, 0:3])
        tv = tab[:, :].rearrange("p (j c) -> p j c", c=4)
        C.ts("dve", idxf[:, :], tv[:, :, 2], -4096.0, 4096.0, ALU.mult, ALU.add)
        C.tt("dve", idxf[:, :], idxf[:, :], tv[:, :, 0], ALU.add)
        C.cp("dve", idxi[:, :], idxf[:, :])
        C.cp("dve", gsl[:, :], tv[:, :, 1])
        if "dbg_idx" in io:
            S.dma("sp", io["dbg_idx"], idxf[:, :])
            S.dma("sp", io["dbg_gsl"], gsl[:, :])
            S.dma("sp", io["dbg_slot"], slotm[:, :])
            S.dma("sp", io["dbg_tab"], tab[:, :])
        XeT = sb("XeT", [128, 16 * CAP], BF16)
        XeTv = XeT[:, :].rearrange("p (c t) -> p c t", t=CAP)
        hTe = sb("hTe", [128, 56 * CAP], BF16)
        hTev = hTe[:, :].rearrange("p (c t) -> p c t", t=CAP)
        xg = [sb("xg%d" % i, [128, D], BF16) for i in range(2)]
        oe = [sb("oe%d" % i, [128, D], F32) for i in range(NCH)]
        sg = [sb("sg%d" % i, [128, CAP], F32) for i in range(2)]
        for i in range(2):
            S.op("dve", lambda e: e.memset(xg[i][:, :], 0.0), writes=[xg[i][:, :]])
        banks = C.psA + C.psB
        bi = 0
        breg = C.nc.gpsimd.alloc_register("bcreg")
        C.nc.gpsimd.reg_mov(breg, TPC - 1)
        N0 = 512
        N1 = CAP - 512
        for e_ in range(8):
            for k in range(NCH):
                g = xg[k % 2]
                j = e_ * NCH + k
                S.dma("pool", g[:, :], io["x2b"], fn=lambda e: e.indirect_dma_start(
                    out=g[:, :], out_offset=None, in_=io["x2b"][:, :],
                    in_offset=bass.IndirectOffsetOnAxis(ap=idxi[:, j:j + 1], axis=0),
                    bounds_check=breg, oob_is_err=False), extra_reads=[idxi[:, j:j + 1]])
                C.to_feature_major(g, XeTv, k * 128)
            for jf in range(28):
                wg = C.slab(io["moe_w_gate"][e_, :, jf * 256:(jf + 1) * 256], 16, 256)
                wu = C.slab(io["moe_w_up"][e_, :, jf * 256:(jf + 1) * 256], 16, 256)
                for m in range(2):
                    pg0, pg1, pu0, pu1 = [banks[(bi + z) % 6] for z in range(4)]
                    bi += 4
                    for c in range(16):
                        C.mm(pg0[:, 0:N0], wg[:, c, m * 128:(m + 1) * 128], XeTv[:, c, 0:N0], c == 0, c == 15)
                        C.mm(pg1[:, 0:N1], wg[:, c, m * 128:(m + 1) * 128], XeTv[:, c, N0:CAP], c == 0, c == 15)
                    for c in range(16):
                        C.mm(pu0[:, 0:N0], wu[:, c, m * 128:(m + 1) * 128], XeTv[:, c, 0:N0], c == 0, c == 15)
                        C.mm(pu1[:, 0:N1], wu[:, c, m * 128:(m + 1) * 128], XeTv[:, c, N0:CAP], c == 0, c == 15)
                    fch = jf * 2 + m
                    sgt = sg[fch % 2]
                    C.act(sgt[:, 0:N0], pg0[:, 0:N0], AF.Silu)
                    C.act(sgt[:, N0:CAP], pg1[:, 0:N1], AF.Silu)
                    C.tt("dve", hTev[:, fch, 0:N0], sgt[:, 0:N0], pu0[:, 0:N0], ALU.mult)
                    C.tt("dve", hTev[:, fch, N0:CAP], sgt[:, N0:CAP], pu1[:, 0:N1], ALU.mult)
            for n in range(4):
                pacc = [banks[(bi + z) % 6] for z in range(NCH)]
                bi += NCH
                for kk in range(7):
                    wd = C.slab(io["moe_w_down"][e_, kk * 1024:(kk + 1) * 1024, n * 512:(n + 1) * 512], 8, 512)
                    for k in range(NCH):
                        for c in range(8):
                            fc = kk * 8 + c
                            C.mm(pacc[k][:, :], hTev[:, fc, k * 128:(k + 1) * 128], wd[:, c, :], fc == 0, fc == 55)
                for k in range(NCH):
                    j = e_ * NCH + k
                    C.ts("dve", oe[k][:, n * 512:(n + 1) * 512], pacc[k][:, :], gsl[:, j:j + 1], None, ALU.mult)
            for k in range(NCH):
                j = e_ * NCH + k
                o_ = oe[k]
                S.dma("pool", io["ybuf"][:, :], o_[:, :], fn=lambda e: e.indirect_dma_start(
                    out=io["ybuf"][:, :], out_offset=bass.IndirectOffsetOnAxis(ap=idxi[:, j:j + 1], axis=0),
                    in_=o_[:, :], in_offset=None, bounds_check=breg, oob_is_err=False, compute_op=ALU.add),
                    extra_reads=[idxi[:, j:j + 1]])
    C.barrier()


def phase_B_final(C, io):
    S = C.S
    es = ExitStack()
    with es:
        def sb(name, shape, dt):
            return es.enter_context(C.nc.sbuf_tensor("sbF_" + name, shape, dt))
        gbc = sb("gbc", [128, D], F32)
        bbc = sb("bbc", [128, D], F32)
        C.bcast_load(gbc[:, :], io["ln_ffn_g"])
        C.bcast_load(bbc[:, :], io["ln_ffn_b"])
        yt = [sb("yt%d" % i, [128, D], F32) for i in range(6)]
        stats = sb("stats", [128, 4, 6], F32)
        mv = sb("mv", [128, 2], F32)
        rstd = sb("rstd", [128, 1], F32)
        for q in range(6):
            S.dma("sp", yt[q][:, :], io["ybuf"][q * 128:(q + 1) * 128, :])
        for q in range(16):
            y = yt[q % 6]
            C.layernorm(y[:, :], gbc[:, :], bbc[:, :], stats, mv, rstd)
            S.dma("pool", io["out"][q * 128:(q + 1) * 128, :], y[:, :])
            if q + 6 < 16:
                S.dma("sp", y[:, :], io["ybuf"][(q + 6) * 128:(q + 7) * 128, :])


def _barrier(self):
    for e in self.S.E:
        self.S.wait_all(e)
    self.S.track = {}


Ctx.barrier = _barrier


def build_fused():
    nc = bass.Bass("TRN2", target_bir_lowering=False)
    es = ExitStack()
    NT = 8
    with es:
        C = Ctx(nc, es)
        io = {}
        io["xc"] = C.dram_in("xc", [2 * (128 + TPC), D])
        io["bands"] = C.dram_in("bands", [24, 128, 128])
        io["identb"] = C.dram_in("identb", [128, 128], BF16)
        io["identf"] = C.dram_in("identf", [128, 128])
        io["perm"] = C.dram_in("perm", [128, 128])
        io["tri"] = C.dram_in("tri", [128, 128], BF16)
        io["ustrict"] = C.dram_in("ustrict", [128, 128], BF16)
        io["iota"] = C.dram_in("iota", [1, CAP])
        io["tokid"] = C.dram_in("tokid", [128, 16])
        io["cosT"] = C.dram_in("cosT", [128, 2 * TPC])
        io["sinT"] = C.dram_in("sinT", [128, 2 * TPC])
        io["validb"] = C.dram_in("validb", [1, 128])
        io["valid01"] = C.dram_in("valid01", [1, 128])
        io["pool_w"] = C.dram_in("pool_w", [4, 512, 512])
        io["pool_scale"] = C.dram_in("pool_scale", [1, D])
        lnA = {k: C.dram_in(k + "0", [1, D]) for k in ("ln_mix_g", "ln_mix_b", "ln_ffn_g", "ln_ffn_b")}
        lnB = {k: C.dram_in(k + "1", [1, D]) for k in ("ln_mix_g", "ln_mix_b", "ln_ffn_g", "ln_ffn_b")}
        io["ffn_w_gate"] = C.dram_in("ffn_w_gate", [D, DFF])
        io["ffn_w_up"] = C.dram_in("ffn_w_up", [D, DFF])
        io["ffn_w_down"] = C.dram_in("ffn_w_down", [DFF, D])
        io["w_kv"] = C.dram_in("w_kv", [D, 2 * D])
        io["moba_wq"] = C.dram_in("moba_wq", [D, D])
        io["moba_wo"] = C.dram_in("moba_wo", [D, D])
        io["moe_router"] = C.dram_in("moe_router", [D, NE])
        io["moe_w_gate"] = C.dram_in("moe_w_gate", [NE, D, DFE])
        io["moe_w_up"] = C.dram_in("moe_w_up", [NE, D, DFE])
        io["moe_w_down"] = C.dram_in("moe_w_down", [NE, DFE, D])
        io["x1"] = C.dram_out("x1", [TPC, D])
        io["kT"] = C.dram_out("kT", [NH, 128, 2 * TPC], BF16)
        io["v"] = C.dram_out("v", [2 * TPC, D], BF16)
        io["x2b"] = C.dram_out("x2b", [TPC, D], BF16)
        io["ybuf"] = C.dram_out("ybuf", [TPC, D])
        io["out"] = C.dram_out("out", [TPC, D])
        C.S.dma("sp", C.identb[:, :], io["identb"])
        C.S.dma("sp", C.identf[:, :], io["identf"])
        st = {"maskf": C.sb("maskf", [128, 128], F32), "maskb": C.sb("maskb", [128, 128], BF16),
              "gate": C.sb("gate", [128, 128], F32), "kms": C.sb("kms", [128, NH * 16], F32)}
        ioA = dict(io)
        ioA.update(lnA)
        phase_A(C, NT, ioA, st["kms"])
        ioB = dict(io)
        ioB.update(lnB)
        phase_B_attn(C, ioB, st)
        phase_B_moe(C, ioB, st)
        phase_B_final(C, ioB)
        C.S.wait_all("sp")
        print("fused: ins", C.S.n_ins, "waits", C.S.n_wait)
    return nc


_CACHE = {}


def _consts():
    import ml_dtypes
    bf = ml_dtypes.bfloat16
    c = {}
    c["identb"] = np.eye(128, dtype=np.float32).astype(bf)
    c["identf"] = np.eye(128, dtype=np.float32)
    c["perm"] = perm_matrix()
    q = np.arange(128)[:, None]
    k = np.arange(128)[None, :]
    c["tri"] = np.where(k <= q, 0.0, NEGM).astype(np.float32).astype(bf)
    c["ustrict"] = (q < k).astype(np.float32).astype(bf)
    c["iota"] = np.arange(CAP, dtype=np.float32)[None, :]
    c["tokid"] = (np.arange(16)[None, :] * 128 + np.arange(128)[:, None]).astype(np.float32)
    return c


def core_inputs(c, x, shared, cst):
    f32 = np.float32
    b, hf = c // 2, c % 2
    segs = [hf * TPC, (1 - hf) * TPC]
    parts = []
    pos = []
    for t0 in segs:
        parts.append(np.zeros((128, D), f32) if t0 == 0 else x[b, t0 - 128:t0])
        parts.append(x[b, t0:t0 + TPC])
        pos.append(np.arange(t0, t0 + TPC))
    cos, sin = rope_tables_T(np.concatenate(pos))
    vb = np.zeros((8, 16), f32)
    v01 = np.zeros((8, 16), f32)
    for lb in range(8):
        for cc in range(16):
            ok = (cc < lb) if cc < 8 else (hf == 1)
            v01[lb, cc] = 1.0 if ok else 0.0
            vb[lb, cc] = 0.0 if ok else -1e30
    m = dict(shared)
    m.update(xc=np.ascontiguousarray(np.concatenate(parts, 0)), bands=make_bands([t0 == 0 for t0 in segs]),
             cosT=cos, sinT=sin, validb=vb.reshape(1, 128), valid01=v01.reshape(1, 128))
    return m


def kernel(x, pool_w, pool_scale, w_kv, moba_wq, moba_wo, ffn_w_gate, ffn_w_up, ffn_w_down,
           moe_router, moe_w_gate, moe_w_up, moe_w_down, ln_mix_g, ln_mix_b, ln_ffn_g, ln_ffn_b):
    f32 = np.float32
    A = lambda a: np.ascontiguousarray(np.asarray(a, dtype=f32))
    x = A(x)
    cst = _consts()
    if "F" not in _CACHE:
        _CACHE["F"] = build_fused()
    nc = _CACHE["F"]
    shared = dict(identb=cst["identb"], identf=cst["identf"], perm=cst["perm"], tri=cst["tri"], ustrict=cst["ustrict"],
                  iota=cst["iota"], tokid=cst["tokid"], pool_w=A(pool_w[0]), pool_scale=A(pool_scale[0:1]),
                  ffn_w_gate=A(ffn_w_gate[0]), ffn_w_up=A(ffn_w_up[0]), ffn_w_down=A(ffn_w_down[0]), w_kv=A(w_kv),
                  moba_wq=A(moba_wq[0]), moba_wo=A(moba_wo[0]), moe_router=A(moe_router[0]),
                  moe_w_gate=A(moe_w_gate[0]), moe_w_up=A(moe_w_up[0]), moe_w_down=A(moe_w_down[0]))
    for k, a in (("ln_mix_g", ln_mix_g), ("ln_mix_b", ln_mix_b), ("ln_ffn_g", ln_ffn_g), ("ln_ffn_b", ln_ffn_b)):
        shared[k + "0"] = A(a[0:1])
        shared[k + "1"] = A(a[1:2])
    in_maps = [core_inputs(c, x, shared, cst) for c in range(NCORE)]
    res = run_bass_kernel_spmd(nc, in_maps, core_ids=list(range(NCORE))).results
    out = np.stack([np.concatenate([res[2 * b]["out"], res[2 * b + 1]["out"]], 0) for b in range(NB_)], 0)
    return out.astype(f32)
```
